# Optimizing a Trainium2 kernel written in Bass

```python
import jax, jax.numpy as jnp
from jax import lax
import numpy as np

D_MODEL = 2048
BATCH = 4
SEQ = 2048
DEPTH = 4

GRID_W = 64
CTX_LEN = 256
N_MIXERS = 2
N_GM_LAYERS = (DEPTH + 1) // 2
N_ML_LAYERS = DEPTH // 2

GM_CHUNK = 128
GM_WIDTH = D_MODEL
GM_GROUPS = 8
GM_GROUP_W = GM_WIDTH // GM_GROUPS

ML_HEADS = 4
ML_DQK = D_MODEL // 8
ML_DV = D_MODEL // 4
ML_QK_W = ML_HEADS * ML_DQK
ML_V_W = ML_HEADS * ML_DV
ML_IN_W = 2 * ML_QK_W + 2 * ML_V_W + 4 * ML_HEADS
ML_CONV_K = 3
ML_CHUNK = 128

N_EXPERTS = 16
N_EXPERT_GROUPS = 4
EXPERTS_PER_GROUP = N_EXPERTS // N_EXPERT_GROUPS
TOP_K = 2
D_EXPERT = D_MODEL // 4

DEEPNORM_ALPHA = (2 * DEPTH) ** 0.25
DEEPNORM_BETA = (8 * DEPTH) ** -0.25
LN_EPS = 1e-5
RMS_EPS = 1e-6

kernel_name = 'hybrid_gmlp_mlstm_groupmoe_deepnorm_dit'


def _layer_norm(x, g, b):
    xf = x.astype(jnp.float32)
    mu = jnp.mean(xf, axis=-1, keepdims=True)
    var = jnp.mean(jnp.square(xf - mu), axis=-1, keepdims=True)
    y = (xf - mu) * lax.rsqrt(var + LN_EPS) * g.astype(jnp.float32) + b.astype(jnp.float32)
    return y.astype(x.dtype)


def _gmlp_mixer(h, w_in, ln_g, ln_b, w_s, b_s, w_out):
    bsz, seq, _ = h.shape
    u, v = jnp.split(jax.nn.gelu(h @ w_in), 2, axis=-1)
    v = _layer_norm(v, ln_g, ln_b).reshape(bsz, seq // GM_CHUNK, GM_CHUNK, GM_GROUPS, GM_GROUP_W)
    s = jnp.einsum('gpq,bnqgc->bnpgc', w_s, v) + b_s.T[:, :, None]
    return (u * s.reshape(bsz, seq, GM_WIDTH)) @ w_out


def _short_conv(x, w):
    pad = ML_CONV_K // 2
    return lax.conv_general_dilated(x, w[:, None, :].astype(x.dtype), window_strides=(1,),
                                    padding=[(pad, pad)], dimension_numbers=('NWC', 'WIO', 'NWC'),
                                    feature_group_count=x.shape[-1])


def _to_heads(t, n_heads):
    bsz, seq, width = t.shape
    return t.reshape(bsz, seq, n_heads, width // n_heads).transpose(0, 2, 1, 3)


def _mlstm_project(h, w_in, conv_w, gate_b):
    f32 = jnp.float32
    p = h @ w_in
    qk = jax.nn.silu(_short_conv(p[..., :2 * ML_QK_W], conv_w))
    v = p[..., 2 * ML_QK_W:2 * ML_QK_W + ML_V_W]
    o = p[..., 2 * ML_QK_W + ML_V_W:2 * ML_QK_W + 2 * ML_V_W]
    g = (p[..., 2 * ML_QK_W + 2 * ML_V_W:].astype(f32) + gate_b.astype(f32)).transpose(0, 2, 1)
    q = _to_heads(qk[..., :ML_QK_W], ML_HEADS).astype(f32)
    k = _to_heads(qk[..., ML_QK_W:], ML_HEADS).astype(f32) * (ML_DQK ** -0.5)
    v = _to_heads(v, ML_HEADS).astype(f32)
    i_fw, f_fw, i_bw, f_bw = jnp.split(g, 4, axis=1)
    gates = ((i_fw, jax.nn.log_sigmoid(f_fw)), (i_bw, jax.nn.log_sigmoid(f_bw)))
    return q, k, v, o, gates


def _mlstm_scan(q, k, v, log_i, log_f, state):
    bsz, nh, seq, _ = q.shape
    dv = v.shape[-1]
    n_chunks = seq // ML_CHUNK

    def chunks(t):
        return jnp.moveaxis(t.reshape(bsz, nh, n_chunks, ML_CHUNK, *t.shape[3:]), 2, 0)

    lower = jnp.tril(jnp.ones((ML_CHUNK, ML_CHUNK), dtype=bool))

    def step(carry, inp):
        c_mat, n_vec, m_prev = carry
        qc, kc, vc, ic, fc = inp
        b = jnp.cumsum(fc, axis=-1)
        d = jnp.where(lower, b[..., :, None] - b[..., None, :] + ic[..., None, :], -jnp.inf)
        inter = b + m_prev[..., None]
        m_t = jnp.maximum(inter, jnp.max(d, axis=-1))
        decay = jnp.exp(inter - m_t)
        s = jnp.einsum('bhtd,bhsd->bhts', qc, kc) * jnp.exp(d - m_t[..., None])
        num = decay[..., None] * jnp.einsum('bhtd,bhde->bhte', qc, c_mat) + jnp.einsum('bhts,bhse->bhte', s, vc)
        den = decay * jnp.einsum('bhtd,bhd->bht', qc, n_vec) + jnp.sum(s, axis=-1)
        h = num / jnp.maximum(jnp.abs(den), jnp.exp(-m_t))[..., None]
        b_last = b[..., -1]
        g = b_last[..., None] - b + ic
        m_new = jnp.maximum(b_last + m_prev, jnp.max(g, axis=-1))
        a = jnp.exp(b_last + m_prev - m_new)
        kw = kc * jnp.exp(g - m_new[..., None])[..., None]
        c_new = a[..., None, None] * c_mat + jnp.einsum('bhsd,bhse->bhde', kw, vc)
        n_new = a[..., None] * n_vec + jnp.sum(kw, axis=2)
        return (c_new, n_new, m_new), h

    state, h = lax.scan(step, state, (chunks(q), chunks(k), chunks(v), chunks(log_i), chunks(log_f)))
    return state, jnp.moveaxis(h, 0, 2).reshape(bsz, nh, seq, dv)


def _mlstm_output(h, o, norm_g, w_out):
    h = h * lax.rsqrt(jnp.mean(jnp.square(h), axis=-1, keepdims=True) + RMS_EPS)
    h = h * norm_g.astype(jnp.float32).reshape(ML_HEADS, 1, ML_DV)
    bsz, nh, seq, dv = h.shape
    h = h.transpose(0, 2, 1, 3).reshape(bsz, seq, nh * dv).astype(o.dtype)
    return (jax.nn.sigmoid(o) * h) @ w_out


def _mlstm_mixer(h_lat, h_ctx, w_in, conv_w, gate_b, norm_g, w_out, col_major, need_ctx):
    bsz, seq, dm = h_lat.shape
    rows = seq // GRID_W
    if col_major:
        h_lat = h_lat.reshape(bsz, rows, GRID_W, dm).transpose(0, 2, 1, 3).reshape(bsz, seq, dm)
    q_l, k_l, v_l, o_l, gates_l = _mlstm_project(h_lat, w_in, conv_w, gate_b)
    q_c, k_c, v_c, o_c, gates_c = _mlstm_project(h_ctx, w_in, conv_w, gate_b)
    zero = (jnp.zeros((bsz, ML_HEADS, ML_DQK, ML_DV), jnp.float32),
            jnp.zeros((bsz, ML_HEADS, ML_DQK), jnp.float32),
            jnp.zeros((bsz, ML_HEADS), jnp.float32))
    h_l, h_c = [], []
    for direction in range(2):
        rev = (lambda t: jnp.flip(t, axis=2)) if direction == 1 else (lambda t: t)
        (i_c, f_c), (i_l, f_l) = gates_c[direction], gates_l[direction]
        ctx_state, hc = _mlstm_scan(rev(q_c), rev(k_c), rev(v_c), rev(i_c), rev(f_c), zero)
        _, hl = _mlstm_scan(rev(q_l), rev(k_l), rev(v_l), rev(i_l), rev(f_l), ctx_state)
        h_l.append(rev(hl))
        h_c.append(rev(hc))
    y_lat = _mlstm_output(h_l[0] + h_l[1], o_l, norm_g, w_out)
    if col_major:
        y_lat = y_lat.reshape(bsz, GRID_W, rows, dm).transpose(0, 2, 1, 3).reshape(bsz, seq, dm)
    y_ctx = _mlstm_output(h_c[0] + h_c[1], o_c, norm_g, w_out) if need_ctx else None
    return y_lat, y_ctx


def _moe(h, router_w, router_b, w_gate, w_up, w_down):
    f32 = jnp.float32
    shape = h.shape
    t = h.reshape(-1, shape[-1])
    logits = (t @ router_w).astype(f32) + router_b.astype(f32)
    probs = jax.nn.softmax(logits, axis=-1)
    pg = probs.reshape(-1, N_EXPERT_GROUPS, EXPERTS_PER_GROUP)
    group_score = jnp.sum(lax.top_k(pg, TOP_K)[0], axis=-1)
    g_sel = jnp.argmax(group_score, axis=-1)
    in_group = jnp.einsum('tge,tg->te', pg, jax.nn.one_hot(g_sel, N_EXPERT_GROUPS, dtype=f32))
    top_p, top_i = lax.top_k(in_group, TOP_K)
    weights = top_p / jnp.sum(top_p, axis=-1, keepdims=True)
    expert_id = g_sel[:, None] * EXPERTS_PER_GROUP + top_i
    gate = jnp.einsum('tke,tk->te', jax.nn.one_hot(expert_id, N_EXPERTS, dtype=f32), weights).astype(h.dtype)
    a = jnp.einsum('td,edf->tef', t, w_gate)
    u = jnp.einsum('td,edf->tef', t, w_up)
    y = jnp.einsum('tef,efd->td', jax.nn.silu(a) * u * gate[:, :, None], w_down)
    return y.reshape(shape)


def setup_inputs(seed: int = 0) -> dict:
    key = jax.random.key(seed)
    ks = jax.random.split(key, 26)

    def nrm(k, shape, scale):
        return jax.random.normal(k, shape, jnp.float32) * scale

    d = D_MODEL
    f_bias = jnp.linspace(3.0, 6.0, ML_HEADS, dtype=jnp.float32)
    f_mask = jnp.array([0.0, 1.0, 0.0, 1.0], jnp.float32)[None, :, None]
    ml_gate_b = (nrm(ks[14], (N_ML_LAYERS, 4, ML_HEADS), 0.1) + f_mask * f_bias[None, None, :]).reshape(N_ML_LAYERS, 4 * ML_HEADS)
    return {
        'x': nrm(ks[0], (BATCH, SEQ, d), 1.0),
        'c': nrm(ks[1], (BATCH, d), 1.0),
        'ctx': nrm(ks[2], (BATCH, CTX_LEN, d), 1.0),
        'c_ctx': nrm(ks[3], (d,), 1.0),
        'ada_w': nrm(ks[4], (DEPTH, d, 6 * d), 0.5 * d ** -0.5),
        'ada_b': nrm(ks[5], (DEPTH, 6 * d), 0.01),
        'gm_w_in': nrm(ks[6], (N_GM_LAYERS, d, 2 * GM_WIDTH), d ** -0.5),
        'gm_ln_g': 1.0 + nrm(ks[7], (N_GM_LAYERS, GM_WIDTH), 0.02),
        'gm_ln_b': nrm(ks[8], (N_GM_LAYERS, GM_WIDTH), 0.02),
        'gm_ws': nrm(ks[9], (N_GM_LAYERS, GM_GROUPS, GM_CHUNK, GM_CHUNK), GM_CHUNK ** -0.5),
        'gm_bs': 1.0 + nrm(ks[10], (N_GM_LAYERS, GM_GROUPS, GM_CHUNK), 0.02),
        'gm_w_out': nrm(ks[11], (N_GM_LAYERS, GM_WIDTH, d), GM_WIDTH ** -0.5 * DEEPNORM_BETA),
        'ml_w_in': nrm(ks[12], (N_ML_LAYERS, d, ML_IN_W), d ** -0.5),
        'ml_conv': nrm(ks[13], (N_ML_LAYERS, ML_CONV_K, 2 * ML_QK_W), ML_CONV_K ** -0.5),
        'ml_gate_b': ml_gate_b,
        'ml_norm_g': 1.0 + nrm(ks[15], (N_ML_LAYERS, ML_V_W), 0.02),
        'ml_w_out': nrm(ks[16], (N_ML_LAYERS, ML_V_W, d), ML_V_W ** -0.5 * DEEPNORM_BETA),
        'ln1_g': 1.0 + nrm(ks[17], (DEPTH, d), 0.02),
        'ln1_b': nrm(ks[18], (DEPTH, d), 0.02),
        'ln2_g': 1.0 + nrm(ks[19], (DEPTH, d), 0.02),
        'ln2_b': nrm(ks[20], (DEPTH, d), 0.02),
        'router_w': nrm(ks[21], (d, N_EXPERTS), d ** -0.5),
        'router_b': nrm(ks[22], (N_EXPERTS,), 0.01),
        'ex_w_gate': nrm(ks[23], (DEPTH, N_EXPERTS, d, D_EXPERT), d ** -0.5),
        'ex_w_up': nrm(ks[24], (DEPTH, N_EXPERTS, d, D_EXPERT), d ** -0.5),
        'ex_w_down': nrm(ks[25], (DEPTH, N_EXPERTS, D_EXPERT, d), D_EXPERT ** -0.5 * DEEPNORM_BETA),
    }


def reference(x, c, ctx, c_ctx, ada_w, ada_b, gm_w_in, gm_ln_g, gm_ln_b, gm_ws, gm_bs, gm_w_out,
              ml_w_in, ml_conv, ml_gate_b, ml_norm_g, ml_w_out, ln1_g, ln1_b, ln2_g, ln2_b,
              router_w, router_b, ex_w_gate, ex_w_up, ex_w_down):
    cond_lat = jax.nn.silu(c)
    cond_ctx = jax.nn.silu(c_ctx)[None, :]
    for layer in range(DEPTH):
        last = layer == DEPTH - 1
        kind = layer % N_MIXERS
        idx = layer // N_MIXERS
        mod_l = (cond_lat @ ada_w[layer] + ada_b[layer])[:, None, :]
        mod_c = (cond_ctx @ ada_w[layer] + ada_b[layer])[:, None, :]
        sh1, sc1, g1, sh2, sc2, g2 = jnp.split(mod_l, 6, axis=-1)
        sh1c, sc1c, g1c, sh2c, sc2c, g2c = jnp.split(mod_c, 6, axis=-1)
        h_lat = x * (1.0 + sc1) + sh1
        if kind == 0:
            gm = (gm_w_in[idx], gm_ln_g[idx], gm_ln_b[idx], gm_ws[idx], gm_bs[idx], gm_w_out[idx])
            y_lat = _gmlp_mixer(h_lat, *gm)
            y_ctx = None if last else _gmlp_mixer(ctx * (1.0 + sc1c) + sh1c, *gm)
        else:
            y_lat, y_ctx = _mlstm_mixer(h_lat, ctx * (1.0 + sc1c) + sh1c, ml_w_in[idx], ml_conv[idx],
                                        ml_gate_b[idx], ml_norm_g[idx], ml_w_out[idx],
                                        col_major=(idx % 2 == 1), need_ctx=not last)
        moe_w = (router_w, router_b, ex_w_gate[layer], ex_w_up[layer], ex_w_down[layer])
        x = _layer_norm(DEEPNORM_ALPHA * x + g1 * y_lat, ln1_g[layer], ln1_b[layer])
        x = _layer_norm(DEEPNORM_ALPHA * x + g2 * _moe(x * (1.0 + sc2) + sh2, *moe_w), ln2_g[layer], ln2_b[layer])
        if not last:
            ctx = _layer_norm(DEEPNORM_ALPHA * ctx + g1c * y_ctx, ln1_g[layer], ln1_b[layer])
            ctx = _layer_norm(DEEPNORM_ALPHA * ctx + g2c * _moe(ctx * (1.0 + sc2c) + sh2c, *moe_w), ln2_g[layer], ln2_b[layer])
    return x
```

```python
import numpy as np
from contextlib import ExitStack
import concourse.bass as bass
import concourse.mybir as mybir
from concourse.bass_utils import run_bass_kernel_spmd

F32 = mybir.dt.float32
BF16 = mybir.dt.bfloat16
AF = mybir.ActivationFunctionType
ALU = mybir.AluOpType
AX = mybir.AxisListType

D = 2048
KC = 16
TL = 2048
TCX = 256
T = TL + TCX
NT = T // 128
DEPTH = 4
ALPHA = float((2 * DEPTH) ** 0.25)
LN_EPS = 1e-5
RMS_EPS = 1e-6
NEXP = 16
DEXP = 512
GROUPS5 = [(0, 512), (512, 512), (1024, 512), (1536, 512), (2048, 256)]


class Res:
    __slots__ = ("name", "w", "rs", "excl")

    def __init__(self, name):
        self.name = name
        self.w = None
        self.rs = {}
        self.excl = False


class Buf:
    def __init__(self, fw, t, name):
        self.fw = fw
        self.t = t
        self.r = Res(name)
        self._ds = None

    @property
    def ds(self):
        if self._ds is None:
            self._ds = self.fw.get_dsem()
        return self._ds

    def __getitem__(self, k):
        return self.t[k]


class FW:
    def __init__(self, nc, es):
        self.nc = nc
        self.es = es
        self.eng = {"pe": nc.tensor, "act": nc.scalar, "dve": nc.vector, "pool": nc.gpsimd, "sp": nc.sync}
        self.semobj = {}
        self.cnt = {}
        for k in ("pe", "act", "dve", "pool"):
            self.semobj[k] = es.enter_context(nc.semaphore("s_" + k))
            self.cnt[k] = 0
        self.seen = {k: {} for k in self.eng}
        self.dpool = []
        self.swmap = {}
        self.dn = 0
        self.uid = 0
        self.live_ds = []

    def get_dsem(self):
        if self.dpool:
            k = self.dpool.pop()
        else:
            self.dn += 1
            k = "d%d" % self.dn
            self.semobj[k] = self.es.enter_context(self.nc.semaphore("s_" + k))
            self.cnt[k] = 0
        self.live_ds.append(k)
        return k

    def _waits(self, e, reads, writes, skip_self=False):
        deps = {}
        for r in reads:
            if r.w is not None:
                k, v = r.w
                if deps.get(k, 0) < v:
                    deps[k] = v
            if r.excl:
                for k, v in r.rs.items():
                    if k != e and deps.get(k, 0) < v:
                        deps[k] = v
        for w in writes:
            if w.w is not None:
                k, v = w.w
                if deps.get(k, 0) < v:
                    deps[k] = v
            for k, v in w.rs.items():
                if deps.get(k, 0) < v:
                    deps[k] = v
        seen = self.seen[e]
        for k, v in deps.items():
            if k == e and (e == "pe" or skip_self):
                continue
            if seen.get(k, 0) >= v:
                continue
            self.eng[e].wait_ge(self.semobj[k], v)
            seen[k] = v

    def _done(self, comp, reads, writes):
        k, v = comp
        for r in reads:
            if r.rs.get(k, 0) < v:
                r.rs[k] = v
        for w in writes:
            w.w = comp
            w.rs = {}

    def op(self, e, fn, reads=(), writes=(), skip_self=False):
        reads = [x.r if isinstance(x, Buf) else x for x in reads]
        writes = [x.r if isinstance(x, Buf) else x for x in writes]
        self._waits(e, reads, writes, skip_self)
        ins = fn(self.eng[e])
        self.cnt[e] += 1
        ins.then_inc(self.semobj[e], 1)
        self._done((e, self.cnt[e]), reads, writes)

    def dma(self, e, pairs, reads=(), writes=(), key=None, **kw):
        reads = [x.r if isinstance(x, Buf) else x for x in reads]
        writes = [x.r if isinstance(x, Buf) else x for x in writes]
        if not isinstance(pairs, list):
            pairs = [pairs]
        if e == "pool":
            if key not in self.swmap:
                self.dn += 1
                k2 = "w%d" % self.dn
                self.semobj[k2] = self.es.enter_context(self.nc.semaphore("s_" + k2))
                self.cnt[k2] = 0
                self.swmap[key] = k2
            key = self.swmap[key]
        self._waits(e, reads, writes)
        for (o, i) in pairs:
            ins = self.eng[e].dma_start(out=o, in_=i, **kw)
            self.cnt[key] += 16
            ins.then_inc(self.semobj[key], 16)
        self._done((key, self.cnt[key]), reads, writes)

    def barrier(self):
        for e in self.eng:
            seen = self.seen[e]
            for k, v in self.cnt.items():
                if v > seen.get(k, 0):
                    self.eng[e].wait_ge(self.semobj[k], v)
                    seen[k] = v


class Phase:
    def __init__(self, fw):
        self.fw = fw
        self.es = ExitStack()
        self.mark = len(fw.live_ds)

    def sb(self, name, shape, dt):
        self.fw.uid += 1
        t = self.es.enter_context(self.fw.nc.sbuf_tensor("%s_%d" % (name, self.fw.uid), shape, dt))
        return Buf(self.fw, t, name)

    def close(self):
        fw = self.fw
        fw.barrier()
        self.es.close()
        rel = fw.live_ds[self.mark:]
        del fw.live_ds[self.mark:]
        fw.dpool.extend(rel)


def build_program(nlayers=DEPTH, debug=False, stop=None):
    nc = bass.Bass("TRN2", target_bir_lowering=False)

    def din(name, shape):
        return nc.dram_tensor(name, list(shape), F32, kind="ExternalInput").ap()

    x_in = din("x", (TL, D))
    c_in = din("c", (1, D))
    ctx_in = din("ctx", (TCX, D))
    cctx_in = din("c_ctx", (1, D))
    ada_w = din("ada_w", (DEPTH, D, 6 * D))
    ada_b = din("ada_b", (DEPTH, 6 * D))
    gm_w_in = din("gm_w_in", (2, D, 2 * D))
    gm_ln_g = din("gm_ln_g", (2, D))
    gm_ln_b = din("gm_ln_b", (2, D))
    gm_ws = din("gm_ws", (2, 8, 128, 128))
    gm_bs = din("gm_bs", (2, 8, 128))
    gm_w_out = din("gm_w_out", (2, D, D))
    ml_w_in = din("ml_w_in", (2, D, 6160))
    ml_conv = din("ml_conv", (2, 3, D))
    ml_gate_b = din("ml_gate_b", (2, 16))
    ml_norm_g = din("ml_norm_g", (2, D))
    ml_w_out = din("ml_w_out", (2, D, D))
    ln1_g = din("ln1_g", (DEPTH, D))
    ln1_b = din("ln1_b", (DEPTH, D))
    ln2_g = din("ln2_g", (DEPTH, D))
    ln2_b = din("ln2_b", (DEPTH, D))
    router_w = din("router_w", (D, NEXP))
    router_b = din("router_b", (1, NEXP))
    ex_w_gate = din("ex_w_gate", (DEPTH, NEXP, D, DEXP))
    ex_w_up = din("ex_w_up", (DEPTH, NEXP, D, DEXP))
    ex_w_down = din("ex_w_down", (DEPTH, NEXP, DEXP, D))
    out = nc.dram_tensor("out", [TL, D], F32, kind="ExternalOutput").ap()

    def scratch(name, shape, dt):
        return nc.dram_tensor(name, list(shape), dt, kind="ExternalOutput" if (debug and name in ("X", "V", "UTs", "XP", "QK", "VS", "ZS")) else "Internal").ap()

    X = scratch("X", (T, D), F32)
    XP = scratch("XP", (T, D), F32)
    V = scratch("V", (T, D), F32)
    UTs = scratch("UTs", (NT, 128, KC * 128), BF16)
    HTs = scratch("HTs", (NT, 128, 64 * 128), BF16)
    QK = scratch("QK", (2048, T), BF16)
    VS = scratch("VS", (T, D), BF16)
    ZS = scratch("ZS", (T, D), BF16)

    with ExitStack() as es:
        fw = FW(nc, es)
        G = Phase(fw)
        pb = []
        for i in range(7):
            pb.append(Buf(fw, es.enter_context(nc.psum_tensor("pb%d" % i, [128, 512], F32)), "pb%d" % i))
        pbh = Buf(fw, es.enter_context(nc.psum_tensor("pbh", [128, 1024], BF16)), "pbh")
        for b_ in pb + [pbh]:
            b_.r.excl = True

        ident = G.sb("ident", [128, 128], F32)
        identb = G.sb("identb", [128, 128], BF16)
        ones32 = G.sb("ones32", [128, 128], F32)
        onesb = G.sb("onesb", [128, 2], BF16)
        condT = G.sb("condT", [128, KC, 2], F32)
        modT = G.sb("modT", [128, 96, 2], F32)
        masklo = G.sb("masklo", [128, 128], F32)
        maskhi = G.sb("maskhi", [128, 128], F32)

        fw.op("pool", lambda g: g.memset(ident[:], 0.0), writes=[ident])
        fw.op("pool", lambda g: g.affine_select(out=ident[:], in_=ident[:], pattern=[[-1, 128]], compare_op=ALU.not_equal, fill=1.0, base=0, channel_multiplier=1), reads=[ident], writes=[ident])
        fw.op("dve", lambda g: g.tensor_copy(out=identb[:], in_=ident[:]), reads=[ident], writes=[identb])
        fw.op("pool", lambda g: g.memset(ones32[:], 1.0), writes=[ones32])
        fw.op("pool", lambda g: g.memset(onesb[:], 1.0), writes=[onesb])
        fw.op("pool", lambda g: g.memset(masklo[:], 1.0), writes=[masklo])
        fw.op("pool", lambda g: g.affine_select(out=masklo[:], in_=masklo[:], pattern=[[1, 128]], compare_op=ALU.is_ge, fill=0.0, base=0, channel_multiplier=-1), reads=[masklo], writes=[masklo])
        fw.op("pool", lambda g: g.memset(maskhi[:], 1.0), writes=[maskhi])
        fw.op("pool", lambda g: g.affine_select(out=maskhi[:], in_=maskhi[:], pattern=[[-1, 128]], compare_op=ALU.is_ge, fill=0.0, base=0, channel_multiplier=1), reads=[maskhi], writes=[maskhi])

        ph = Phase(fw)
        csb = ph.sb("csb", [16, 2, 128], F32)
        fw.dma("sp", (csb[:, 0, :], c_in.rearrange("o (k p) -> (o k) p", p=128)), writes=[csb], key=csb.ds)
        fw.dma("sp", (csb[:, 1, :], cctx_in.rearrange("o (k p) -> (o k) p", p=128)), writes=[csb], key=csb.ds)
        fw.op("act", lambda g: g.activation(out=csb[:], in_=csb[:], func=AF.Silu), reads=[csb], writes=[csb])
        for r in range(2):
            fw.op("pe", lambda g: g.transpose(pb[0][:, r * 16:(r + 1) * 16], csb[:, r, :], ident[0:16, 0:16]), reads=[csb, ident], writes=[pb[0]])
        for r in range(2):
            fw.op("dve", lambda g: g.tensor_copy(out=condT[:, :, r], in_=pb[0][:, r * 16:(r + 1) * 16]), reads=[pb[0]], writes=[condT])
        cpk = fw.get_dsem()
        xres = Res("Xdram")
        fw.dma("sp", [(X[0:TL, :], x_in[:, :]), (X[TL:T, :], ctx_in[:, :])], writes=[xres], key=cpk)
        ph.close()

        def xrows(A, l, i, c0=0, c1=D):
            if i >= 16 or l != 3:
                return [(slice(0, 128), A[i * 128:(i + 1) * 128, c0:c1])]
            v = A[0:TL, c0:c1].rearrange("(r w) d -> w r d", w=64)
            return [(slice(wl * 32, (wl + 1) * 32), v[i * 4 + wl]) for wl in range(4)]

        def load_rows(e, buf, A, l, i, c0=0, c1=D, ap=None):
            dst = buf.t if ap is None else ap
            fw.dma(e, [(dst[ps, :] if ap is None else ap[ps], src) for ps, src in xrows(A, l, i, c0, c1)], writes=[buf], key=buf.ds)

        def store_rows(e, buf, A, l, i, c0=0, c1=D, srcap=None):
            fw.dma(e, [(dst, (buf.t if srcap is None else srcap)[ps]) for ps, dst in xrows(A, l, i, c0, c1)], reads=[buf], key=buf.ds)

        def phase_ada(l):
            ph = Phase(fw)
            for _ in gen_ada(l, ph):
                pass
            ph.close()

        def gen_ada(l, ph):
            abt = ph.sb("abt", [96, 128], F32)
            abT = ph.sb("abT", [128, 96], F32)
            aw = [ph.sb("aw%d" % i, [128, KC, 512], BF16) for i in range(3)]
            condTb = ph.sb("condTb", [128, KC, 2], BF16)
            fw.op("dve", lambda g: g.tensor_copy(out=condTb[:], in_=condT[:]), reads=[condT], writes=[condTb])
            fw.dma("sp", (abt[:], ada_b[l].rearrange("(c p) -> c p", p=128)), writes=[abt], key=abt.ds)
            fw.op("pe", lambda g: g.transpose(pb[1][:, 0:96], abt[:], ident[0:96, 0:96]), reads=[abt, ident], writes=[pb[1]])
            fw.op("dve", lambda g: g.tensor_copy(out=abT[:], in_=pb[1][:, 0:96]), reads=[pb[1]], writes=[abT])
            awv = ada_w[l].rearrange("(k p) n -> p k n", p=128)
            for j in range(24):
                a = aw[j % 3]
                fw.dma("pool", (a[:], awv[:, :, j * 512:(j + 1) * 512]), writes=[a], key=a.ds)
                for c2 in range(4):
                    cidx = j * 4 + c2
                    for k in range(KC):
                        fw.op("pe", lambda g: g.matmul(pb[0][:, 2 * cidx:2 * cidx + 2], lhsT=a[:, k, c2 * 128:(c2 + 1) * 128], rhs=condTb[:, k, :], start=(k == 0), stop=(k == KC - 1)), reads=[a, condTb], writes=[pb[0]])
                yield
            pv = pb[0][:, 0:192].rearrange("p (c r) -> p c r", r=2)
            for r in range(2):
                fw.op("dve", lambda g: g.tensor_tensor(out=modT[:, :, r], in0=pv[:, :, r], in1=abT[:], op=ALU.add), reads=[pb[0], abT], writes=[modT])
            for c0 in (16, 64):
                fw.op("dve", lambda g: g.tensor_scalar_add(out=modT[:, c0:c0 + 16, :], in0=modT[:, c0:c0 + 16, :], scalar1=1.0), reads=[modT], writes=[modT])

        def build_gb(gB, cidx0):
            ph = Phase(fw)
            dg = [ph.sb("dg%d" % i, [128, 128], F32) for i in range(2)]
            n = 0
            for r in range(2):
                for q in range(4):
                    bank = pb[1 + (n % 2)]
                    n += 1
                    for cc in range(4):
                        c = q * 4 + cc
                        d_ = dg[c % 2]
                        fw.op("dve", lambda g: g.tensor_scalar(out=d_[:], in0=ident[:], scalar1=modT[:, cidx0 + c, r:r + 1], scalar2=None, op0=ALU.mult), reads=[ident, modT], writes=[d_])
                        fw.op("pe", lambda g: g.matmul(bank[:, cc * 128:(cc + 1) * 128], lhsT=ones32[:], rhs=d_[:], start=True, stop=True), reads=[ones32, d_], writes=[bank])
                    fw.op("act", lambda g: g.copy(out=gB[r][:, q * 512:(q + 1) * 512], in_=bank[:]), reads=[bank], writes=[gB[r]])
            ph.close()

        def phase_xt(l, hT, hres, sh0, sc0, router=None):
            ph = Phase(fw)
            xt = [ph.sb("xt%d" % i, [128, D], F32) for i in range(3)]
            if router is not None:
                rw = ph.sb("rw", [128, KC, NEXP], F32)
                rbB = ph.sb("rbB", [128, NEXP], F32)
                xm32 = [ph.sb("xm32_%d" % i, [128, KC, 128], F32) for i in range(2)]
                gs = {n_: [ph.sb("%s%d" % (n_, i), s_, F32) for i in range(2)] for n_, s_ in (("lg", [128, 16]), ("ee", [128, 16]), ("tt", [128, 16]), ("gt", [128, 16]), ("m1", [128, 4]), ("m2", [128, 4]), ("sc", [128, 4]), ("gm", [128, 4]), ("c1", [128, 4]))}
                import os as _os
                XB = int(_os.environ.get("XB", "0"))
                if not (XB & 2):
                    fw.dma("sp", (rw[:], router_w.rearrange("(k p) e -> p k e", p=128)), writes=[rw], key=rw.ds)
                    fw.dma("sp", (rbB[:], router_b[0].partition_broadcast(128)), writes=[rbB], key=rbB.ds)
                gateT = router
            ntile = NT
            if router is not None and (XB & 4):
                ntile = 0
            if ntile > 0:
                load_rows("sp", xt[0], X, l, 0)
            for i in range(ntile):
                r = 0 if i < 16 else 1
                if i + 1 < ntile:
                    load_rows("sp", xt[(i + 1) % 3], X, l, i + 1)
                x_ = xt[i % 3]
                ev = "act" if i % 2 == 0 else "dve"
                for q in range(4):
                    bank = pb[(i % 2) * 2 + (q % 2)]
                    for cc in range(4):
                        c = q * 4 + cc
                        fw.op("pe", lambda g: g.transpose(bank[:, cc * 128:(cc + 1) * 128], x_[:, c * 128:(c + 1) * 128], ident[:]), reads=[x_, ident], writes=[bank])
                    for cc in range(4):
                        c = q * 4 + cc
                        src = bank[:, cc * 128:(cc + 1) * 128]
                        dst = hT[:, c, i * 128:(i + 1) * 128]
                        scp = modT[:, sc0 + c, r:r + 1]
                        shp = modT[:, sh0 + c, r:r + 1]
                        if ev == "act":
                            fw.op("act", lambda g: g.activation(out=dst, in_=src, func=AF.Identity, bias=shp, scale=scp), reads=[bank, modT], writes=[hres[i]], skip_self=True)
                        else:
                            fw.op("dve", lambda g: g.tensor_scalar(out=dst, in0=src, scalar1=scp, scalar2=shp, op0=ALU.mult, op1=ALU.add), reads=[bank, modT], writes=[hres[i]], skip_self=True)
                        if router is not None:
                            xm = xm32[i % 2]
                            ev2 = "dve" if ev == "act" else "act"
                            d2 = xm[:, c, :]
                            if ev2 == "act":
                                fw.op("act", lambda g: g.activation(out=d2, in_=src, func=AF.Identity, bias=shp, scale=scp), reads=[bank, modT], writes=[xm], skip_self=True)
                            else:
                                fw.op("dve", lambda g: g.tensor_scalar(out=d2, in0=src, scalar1=scp, scalar2=shp, op0=ALU.mult, op1=ALU.add), reads=[bank, modT], writes=[xm], skip_self=True)
                import os as _os
                RM = int(_os.environ.get("RM", "3"))
                if router is not None and RM >= 1:
                    xm = xm32[i % 2]
                    bk = pb[4 + (i % 2)]
                    for k in range(KC):
                        fw.op("pe", lambda g: g.matmul(bk[:, 0:16], lhsT=xm[:, k, :], rhs=rw[:, k, :], start=(k == 0), stop=(k == KC - 1)), reads=[xm, rw], writes=[bk])
                    s = {n_: v_[i % 2] for n_, v_ in gs.items()}
                    V_ = "dve"
                    fw.op(V_, lambda g: g.tensor_tensor(out=s["lg"][:], in0=bk[:, 0:16], in1=rbB[:], op=ALU.add), reads=[bk, rbB], writes=[s["lg"]])
                    if RM < 2:
                        continue
                    fw.op(V_, lambda g: g.reduce_max(out=s["c1"][:, 0:1], in_=s["lg"][:], axis=AX.X), reads=[s["lg"]], writes=[s["c1"]])
                    fw.op(V_, lambda g: g.tensor_scalar(out=s["c1"][:, 1:2], in0=s["c1"][:, 0:1], scalar1=-1.0, scalar2=None, op0=ALU.mult), reads=[s["c1"]], writes=[s["c1"]])
                    fw.op("act", lambda g: g.activation(out=s["ee"][:], in_=s["lg"][:], func=AF.Exp, bias=s["c1"][:, 1:2], scale=1.0), reads=[s["lg"], s["c1"]], writes=[s["ee"]])
                    e3 = s["ee"][:].rearrange("p (g e) -> p g e", e=4)
                    t3 = s["tt"][:].rearrange("p (g e) -> p g e", e=4)
                    fw.op(V_, lambda g: g.tensor_reduce(out=s["m1"][:], in_=e3, axis=AX.X, op=ALU.max), reads=[s["ee"]], writes=[s["m1"]])
                    fw.op(V_, lambda g: g.tensor_tensor(out=t3, in0=e3, in1=s["m1"][:].unsqueeze(2).to_broadcast([128, 4, 4]), op=ALU.is_lt), reads=[s["ee"], s["m1"]], writes=[s["tt"]])
                    fw.op(V_, lambda g: g.tensor_tensor(out=s["tt"][:], in0=s["tt"][:], in1=s["ee"][:], op=ALU.mult), reads=[s["tt"], s["ee"]], writes=[s["tt"]])
                    fw.op(V_, lambda g: g.tensor_reduce(out=s["m2"][:], in_=t3, axis=AX.X, op=ALU.max), reads=[s["tt"]], writes=[s["m2"]])
                    fw.op(V_, lambda g: g.tensor_tensor(out=s["sc"][:], in0=s["m1"][:], in1=s["m2"][:], op=ALU.add), reads=[s["m1"], s["m2"]], writes=[s["sc"]])
                    fw.op(V_, lambda g: g.reduce_max(out=s["c1"][:, 2:3], in_=s["sc"][:], axis=AX.X), reads=[s["sc"]], writes=[s["c1"]])
                    fw.op(V_, lambda g: g.tensor_scalar(out=s["gm"][:], in0=s["sc"][:], scalar1=s["c1"][:, 2:3], scalar2=None, op0=ALU.is_ge), reads=[s["sc"], s["c1"]], writes=[s["gm"]])
                    fw.op(V_, lambda g: g.tensor_tensor(out=t3, in0=e3, in1=s["m2"][:].unsqueeze(2).to_broadcast([128, 4, 4]), op=ALU.is_ge), reads=[s["ee"], s["m2"]], writes=[s["tt"]])
                    fw.op(V_, lambda g: g.tensor_tensor(out=t3, in0=t3, in1=s["gm"][:].unsqueeze(2).to_broadcast([128, 4, 4]), op=ALU.mult), reads=[s["tt"], s["gm"]], writes=[s["tt"]])
                    fw.op(V_, lambda g: g.reciprocal(out=s["c1"][:, 3:4], in_=s["c1"][:, 2:3]), reads=[s["c1"]], writes=[s["c1"]])
                    fw.op(V_, lambda g: g.scalar_tensor_tensor(out=s["gt"][:], in0=s["ee"][:], scalar=s["c1"][:, 3:4], in1=s["tt"][:], op0=ALU.mult, op1=ALU.mult), reads=[s["ee"], s["c1"], s["tt"]], writes=[s["gt"]])
                    if RM < 3:
                        continue
                    fw.op("pe", lambda g: g.transpose(bk[0:16, 128:256], s["gt"][:], ident[:]), reads=[s["gt"], ident], writes=[bk])
                    fw.op("act", lambda g: g.copy(out=gateT[0:16, i * 128:(i + 1) * 128], in_=bk[0:16, 128:256]), reads=[bk], writes=[gateT])
            ph.close()

        def resid_ln(gB, ph_bufs, l, i, ybanks, lng, lnb, dstA, last_lat_out=False):
            xt, yt, junk, st = ph_bufs
            r = 0 if i < 16 else 1
            for q in range(4):
                sl = slice(q * 512, (q + 1) * 512)
                fw.op("dve", lambda g: g.tensor_tensor(out=yt[:, sl], in0=ybanks[q][:], in1=gB[r][:, sl], op=ALU.mult), reads=[ybanks[q], gB[r]], writes=[yt])
            if debug:
                store_rows("sp", yt, XP, l, i)
            fw.op("dve", lambda g: g.scalar_tensor_tensor(out=xt[:], in0=xt[:], scalar=ALPHA, in1=yt[:], op0=ALU.mult, op1=ALU.add), reads=[xt, yt], writes=[xt])
            layer_norm(xt, junk, st, lng, lnb, yt)
            dst = out if (last_lat_out and i < 16) else dstA
            store_rows("sp", yt, dst, l, i)

        def layer_norm(xt, junk, st, lng, lnb, outb, width=D, eps=LN_EPS, out_ap=None, in_ap=None):
            xin = xt[:] if in_ap is None else in_ap
            fw.op("dve", lambda g: g.reduce_sum(out=st[:, 0:1], in_=xin, axis=AX.X), reads=[xt], writes=[st])
            fw.op("dve", lambda g: g.tensor_scalar(out=st[:, 1:2], in0=st[:, 0:1], scalar1=-1.0 / width, scalar2=None, op0=ALU.mult), reads=[st], writes=[st])
            fw.op("act", lambda g: g.activation(out=junk[:, 0:width], in_=xin, func=AF.Square, bias=st[:, 1:2], scale=1.0, accum_out=st[:, 2:3]), reads=[xt, st], writes=[junk, st])
            fw.op("dve", lambda g: g.tensor_scalar(out=st[:, 3:4], in0=st[:, 2:3], scalar1=1.0 / width, scalar2=eps, op0=ALU.mult, op1=ALU.add), reads=[st], writes=[st])
            fw.op("act", lambda g: g.activation(out=st[:, 4:5], in_=st[:, 3:4], func=AF.Sqrt), reads=[st], writes=[st])
            fw.op("dve", lambda g: g.reciprocal(out=st[:, 5:6], in_=st[:, 4:5]), reads=[st], writes=[st])
            fw.op("dve", lambda g: g.tensor_scalar(out=xin, in0=xin, scalar1=st[:, 1:2], scalar2=st[:, 5:6], op0=ALU.add, op1=ALU.mult), reads=[xt, st], writes=[xt])
            oap = outb[:] if out_ap is None else out_ap
            fw.op("pool", lambda g: g.tensor_tensor(out=xin, in0=xin, in1=lng[:, 0:width], op=ALU.mult), reads=[xt, lng], writes=[xt])
            fw.op("pool", lambda g: g.tensor_tensor(out=oap, in0=xin, in1=lnb[:, 0:width], op=ALU.add), reads=[xt, lnb], writes=[outb])

        def phase_gmlp(l, idx):
            last = l == DEPTH - 1
            ph = Phase(fw)
            hT = ph.sb("hT", [128, KC, T], BF16)
            hres = [Res("hT%d" % i) for i in range(NT)]
            phase_xt(l, hT, hres, 0, 16)
            wb = [ph.sb("wb%d" % i, [128, KC, 512], BF16) for i in range(2)]
            osb = [ph.sb("osb%d" % i, [128, 512], BF16) for i in range(3)]
            vsb = [ph.sb("vsb%d" % i, [128, 512], F32) for i in range(3)]
            wv = gm_w_in[idx].rearrange("(k p) n -> p k n", p=128)
            nb = 0
            no = 0
            for blk in range(8):
                w = wb[blk % 2]
                fw.dma("pool", (w[:], wv[:, :, blk * 512:(blk + 1) * 512]), writes=[w], key=w.ds)
                if blk < 4:
                    for (t0, n) in GROUPS5:
                        for c4 in range(4):
                            bank = pb[nb % 6]
                            nb += 1
                            for k in range(KC):
                                fw.op("pe", lambda g: g.matmul(bank[:, 0:n], lhsT=w[:, k, c4 * 128:(c4 + 1) * 128], rhs=hT[:, k, t0:t0 + n], start=(k == 0), stop=(k == KC - 1)), reads=[w] + hres[t0 // 128:(t0 + n) // 128], writes=[bank])
                            o = osb[no % 3]
                            no += 1
                            fw.op("act", lambda g: g.activation(out=o[:, 0:n], in_=bank[:, 0:n], func=AF.Gelu_apprx_tanh), reads=[bank], writes=[o])
                            ch = blk * 4 + c4
                            nt_ = n // 128
                            fw.dma("sp", (UTs[t0 // 128:t0 // 128 + nt_, :, ch * 128:(ch + 1) * 128].rearrange("t p n -> p t n"), o[:, 0:n].rearrange("p (t n) -> p t n", n=128)), reads=[o], key=o.ds)
                else:
                    for i in range(NT):
                        bank = pb[nb % 6]
                        nb += 1
                        for k in range(KC):
                            fw.op("pe", lambda g: g.matmul(bank[:], lhsT=hT[:, k, i * 128:(i + 1) * 128], rhs=w[:, k, :], start=(k == 0), stop=(k == KC - 1)), reads=[w, hres[i]], writes=[bank])
                        o = vsb[no % 3]
                        no += 1
                        fw.op("act", lambda g: g.activation(out=o[:], in_=bank[:], func=AF.Gelu_apprx_tanh), reads=[bank], writes=[o])
                        fw.dma("sp", (V[i * 128:(i + 1) * 128, (blk - 4) * 512:(blk - 3) * 512], o[:]), reads=[o], key=o.ds)
            ph.close()

            ph = Phase(fw)
            gB = [ph.sb("gB%d" % i, [128, D], F32) for i in range(2)]
            build_gb(gB, 32)
            wo = ph.sb("wo", [128, KC, D], BF16)
            wov = gm_w_out[idx].rearrange("(k p) n -> p k n", p=128)
            woh = [Res("wo%d" % q) for q in range(4)]
            for q in range(4):
                fw.dma("pool", (wo[:, :, q * 512:(q + 1) * 512], wov[:, :, q * 512:(q + 1) * 512]), writes=[woh[q]], key=fw.get_dsem())
            lgB = ph.sb("lgB", [128, D], F32)
            lbB = ph.sb("lbB", [128, D], F32)
            l1g = ph.sb("l1g", [128, D], F32)
            l1b = ph.sb("l1b", [128, D], F32)
            bsB = ph.sb("bsB", [128, KC, 128], F32)
            wst = ph.sb("wst", [128, 8, 128], F32)
            wsT = ph.sb("wsT", [128, 8, 128], BF16)
            fw.dma("sp", (lgB[:], gm_ln_g[idx].partition_broadcast(128)), writes=[lgB], key=lgB.ds)
            fw.dma("sp", (lbB[:], gm_ln_b[idx].partition_broadcast(128)), writes=[lbB], key=lbB.ds)
            fw.dma("sp", (l1g[:], ln1_g[l].partition_broadcast(128)), writes=[l1g], key=l1g.ds)
            fw.dma("sp", (l1b[:], ln1_b[l].partition_broadcast(128)), writes=[l1b], key=l1b.ds)
            bsv = gm_bs[idx].rearrange("g p -> (g p)").partition_broadcast(128).rearrange("q (g p) -> q g p", p=128)
            for dup in range(2):
                fw.dma("sp", (bsB[:].rearrange("q (g d) p -> q g d p", d=2)[:, :, dup, :], bsv), writes=[bsB], key=bsB.ds)
            fw.dma("sp", (wst[:], gm_ws[idx].rearrange("g p q -> p g q")), writes=[wst], key=wst.ds)
            for g_ in range(8):
                bank = pb[g_ % 2]
                fw.op("pe", lambda g: g.transpose(bank[:, 0:128], wst[:, g_, :], ident[:]), reads=[wst, ident], writes=[bank])
                fw.op("dve", lambda g: g.tensor_copy(out=wsT[:, g_, :], in_=bank[:, 0:128]), reads=[bank], writes=[wsT])
            vt = [ph.sb("vt%d" % i, [128, D], F32) for i in range(2)]
            ut = [ph.sb("ut%d" % i, [128, KC, 128], BF16) for i in range(2)]
            xt = [ph.sb("xt%d" % i, [128, D], F32) for i in range(2)]
            yt = [ph.sb("yt%d" % i, [128, D], F32) for i in range(1)] * 2
            vn = [ph.sb("vn%d" % i, [128, D], BF16) for i in range(2)]
            zT = [ph.sb("zT%d" % i, [128, KC, 128], BF16) for i in range(2)]
            stt = [ph.sb("st%d" % i, [128, 8], F32) for i in range(2)]
            st2 = [ph.sb("stb%d" % i, [128, 8], F32) for i in range(2)]
            tmp = [ph.sb("tmp%d" % i, [128, 512], F32) for i in range(2)]
            jkv = ph.sb("jkv", [128, D], BF16)
            ntile = 16 if last else NT

            def loads(i):
                fw.dma("sp", (vt[i % 2][:], V[i * 128:(i + 1) * 128, :]), writes=[vt[i % 2]], key=vt[i % 2].ds)
                fw.dma("sp", (ut[i % 2][:].rearrange("p k n -> p (k n)"), UTs[i]), writes=[ut[i % 2]], key=ut[i % 2].ds)
                load_rows("sp", xt[i % 2], X, l, i)
            loads(0)
            nb = 0
            for i in range(ntile):
                if i + 1 < ntile:
                    loads(i + 1)
                v_, u_, x_, y_, n_, z_ = vt[i % 2], ut[i % 2], xt[i % 2], yt[i % 2], vn[i % 2], zT[i % 2]
                layer_norm(v_, jkv, stt[i % 2], lgB, lbB, n_)
                for q in range(4):
                    bank = pb[q % 2]
                    for cc in range(4):
                        c = q * 4 + cc
                        fw.op("pe", lambda g: g.matmul(bank[:, cc * 128:(cc + 1) * 128], lhsT=n_[:, c * 128:(c + 1) * 128], rhs=wsT[:, c // 2, :], start=True, stop=True), reads=[n_, wsT], writes=[bank])
                    tm = tmp[q % 2]
                    fw.op("dve", lambda g: g.tensor_tensor(out=tm[:], in0=bank[:], in1=bsB[:, q * 4:(q + 1) * 4, :].rearrange("p c n -> p (c n)"), op=ALU.add), reads=[bank, bsB], writes=[tm])
                    fw.op("pool", lambda g: g.tensor_tensor(out=z_[:, q * 4:(q + 1) * 4, :].rearrange("p c n -> p (c n)"), in0=tm[:], in1=u_[:, q * 4:(q + 1) * 4, :].rearrange("p c n -> p (c n)"), op=ALU.mult), reads=[tm, u_], writes=[z_])
                yb = []
                for q in range(4):
                    bank = pb[2 + (nb % 5)]
                    nb += 1
                    for k in range(KC):
                        fw.op("pe", lambda g: g.matmul(bank[:], lhsT=z_[:, k, :], rhs=wo[:, k, q * 512:(q + 1) * 512], start=(k == 0), stop=(k == KC - 1)), reads=[z_, woh[q]], writes=[bank])
                    yb.append(bank)
                resid_ln(gB, (x_, y_, v_, st2[i % 2]), l, i, yb, l1g, l1b, X)
            ph.close()

        def phase_moe(l):
            last = l == DEPTH - 1
            ntile = 16 if last else NT
            groups = GROUPS5[:4] if last else GROUPS5
            ph = Phase(fw)
            sel16 = ph.sb("sel16", [16, 16, 128], F32)
            import os as _os
            XB = int(_os.environ.get("XB", "0"))
            if not (XB & 1):
                fw.op("pool", lambda g: g.memset(sel16[:], 0.0), writes=[sel16])
                fw.op("pool", lambda g: g.affine_select(out=sel16[:], in_=sel16[:], pattern=[[-1, 16], [0, 128]], compare_op=ALU.not_equal, fill=1.0, base=0, channel_multiplier=1), reads=[sel16], writes=[sel16])
            hT = ph.sb("hT", [128, KC, T], BF16)
            hres = [Res("hT%d" % i) for i in range(NT)]
            gateT = ph.sb("gateT", [16, T], F32)
            phase_xt(l, hT, hres, 48, 64, router=gateT)
            if stop == ("moe_xt", l):
                ph.close()
                return
            wg = [ph.sb("wg%d" % i, [128, KC, 512], BF16) for i in range(2)]
            wu = [ph.sb("wu%d" % i, [128, KC, 512], BF16) for i in range(2)]
            gbs = [ph.sb("gbs%d" % i, [128, 512], F32) for i in range(2)]
            sa = [ph.sb("sa%d" % i, [128, 512], F32) for i in range(2)]
            hb = [ph.sb("hb%d" % i, [128, 512], BF16) for i in range(3)]
            nh = 0
            ng = 0
            for e in range(NEXP):
                g_, u_ = wg[e % 2], wu[e % 2]
                fw.dma("pool", (g_[:], ex_w_gate[l, e].rearrange("(k p) n -> p k n", p=128)), writes=[g_], key=g_.ds)
                fw.dma("pool", (u_[:], ex_w_up[l, e].rearrange("(k p) n -> p k n", p=128)), writes=[u_], key=u_.ds)
                for (t0, n) in groups:
                    gbk = pb[6]
                    gb_ = gbs[ng % 2]
                    ng += 1
                    fw.op("pe", lambda g: g.matmul(gbk[:, 0:n], lhsT=sel16[:, e, :], rhs=gateT[0:16, t0:t0 + n], start=True, stop=True), reads=[sel16, gateT], writes=[gbk])
                    fw.op("act", lambda g: g.copy(out=gb_[:, 0:n], in_=gbk[:, 0:n]), reads=[gbk], writes=[gb_])
                    for fc in range(4):
                        ba = pb[(nh % 3) * 2]
                        bu = pb[(nh % 3) * 2 + 1]
                        for k in range(KC):
                            fw.op("pe", lambda g: g.matmul(ba[:, 0:n], lhsT=g_[:, k, fc * 128:(fc + 1) * 128], rhs=hT[:, k, t0:t0 + n], start=(k == 0), stop=(k == KC - 1)), reads=[g_] + hres[t0 // 128:(t0 + n) // 128], writes=[ba])
                        for k in range(KC):
                            fw.op("pe", lambda g: g.matmul(bu[:, 0:n], lhsT=u_[:, k, fc * 128:(fc + 1) * 128], rhs=hT[:, k, t0:t0 + n], start=(k == 0), stop=(k == KC - 1)), reads=[u_] + hres[t0 // 128:(t0 + n) // 128], writes=[bu])
                        s_ = sa[nh % 2]
                        h_ = hb[nh % 3]
                        nh += 1
                        fw.op("act", lambda g: g.activation(out=s_[:, 0:n], in_=ba[:, 0:n], func=AF.Silu), reads=[ba], writes=[s_])
                        fw.op("pool", lambda g: g.tensor_tensor(out=s_[:, 0:n], in0=s_[:, 0:n], in1=gb_[:, 0:n], op=ALU.mult), reads=[s_, gb_], writes=[s_])
                        fw.op("dve", lambda g: g.tensor_tensor(out=h_[:, 0:n], in0=bu[:, 0:n], in1=s_[:, 0:n], op=ALU.mult), reads=[bu, s_], writes=[h_])
                        ch = e * 4 + fc
                        nt_ = n // 128
                        fw.dma("sp", (HTs[t0 // 128:t0 // 128 + nt_, :, ch * 128:(ch + 1) * 128].rearrange("t p n -> p t n"), h_[:, 0:n].rearrange("p (t n) -> p t n", n=128)), reads=[h_], key=h_.ds)
            ph.close()

            if stop == ("moe_m2", l):
                return
            ph = Phase(fw)
            gB = [ph.sb("gB%d" % i, [128, D], F32) for i in range(2)]
            build_gb(gB, 80)
            wd = [ph.sb("wd%d" % i, [128, 32, 512], BF16) for i in range(2)]
            ht = [ph.sb("ht%d" % i, [128, 64, 128], BF16) for i in range(3)]
            xq = [ph.sb("xq%d" % i, [128, 512], F32) for i in range(3)]
            yq = [ph.sb("yq%d" % i, [128, 512], F32) for i in range(3)]
            wdv = ex_w_down[l].rearrange("e (fc p) n -> p (e fc) n", p=128)
            n = 0
            seq = [(dt_, i) for dt_ in range(4) for i in range(ntile)]

            def m3_loads(j):
                dt2, i2 = seq[j]
                fw.dma("sp", (ht[j % 3][:].rearrange("p k n -> p (k n)"), HTs[i2]), writes=[ht[j % 3]], key=ht[j % 3].ds)
                load_rows("sp", xq[j % 3], X, l, i2, dt2 * 512, (dt2 + 1) * 512)
            m3_loads(0)
            for dt_ in range(4):
                sl = slice(dt_ * 512, (dt_ + 1) * 512)
                for hf in range(2):
                    fw.dma("pool", (wd[hf][:], wdv[:, hf * 32:(hf + 1) * 32, sl]), writes=[wd[hf]], key=wd[hf].ds)
                for i in range(ntile):
                    r = 0 if i < 16 else 1
                    h_ = ht[n % 3]
                    x_ = xq[n % 3]
                    y_ = yq[n % 3]
                    bank = pb[n % 6]
                    n += 1
                    if n < len(seq):
                        m3_loads(n)
                    for j in range(64):
                        fw.op("pe", lambda g: g.matmul(bank[:], lhsT=h_[:, j, :], rhs=wd[j // 32][:, j % 32, :], start=(j == 0), stop=(j == 63)), reads=[h_, wd[j // 32]], writes=[bank])
                    fw.op("dve", lambda g: g.tensor_tensor(out=y_[:], in0=bank[:], in1=gB[r][:, sl], op=ALU.mult), reads=[bank, gB[r]], writes=[y_])
                    fw.op("dve", lambda g: g.scalar_tensor_tensor(out=y_[:], in0=x_[:], scalar=ALPHA, in1=y_[:], op0=ALU.mult, op1=ALU.add), reads=[x_, y_], writes=[y_])
                    store_rows("sp", y_, XP, l, i, dt_ * 512, (dt_ + 1) * 512)
            ph.close()

            if stop == ("moe_m3", l):
                return
            ph = Phase(fw)
            gens = [gen_ln2(l, ph, ntile, last)]
            if l + 1 < nlayers:
                gens.append(gen_ada(l + 1, ph))
            while gens:
                for g_ in list(gens):
                    try:
                        next(g_)
                    except StopIteration:
                        gens.remove(g_)
            ph.close()

        def gen_ln2(l, ph, ntile, last):
            l2g = ph.sb("l2g", [128, D], F32)
            l2b = ph.sb("l2b", [128, D], F32)
            fw.dma("sp", (l2g[:], ln2_g[l].partition_broadcast(128)), writes=[l2g], key=l2g.ds)
            fw.dma("sp", (l2b[:], ln2_b[l].partition_broadcast(128)), writes=[l2b], key=l2b.ds)
            xt = [ph.sb("xt%d" % i, [128, D], F32) for i in range(3)]
            yt = [ph.sb("yt%d" % i, [128, D], F32) for i in range(2)]
            jk = ph.sb("jk", [128, D], BF16)
            stt = [ph.sb("st%d" % i, [128, 8], F32) for i in range(2)]
            load_rows("sp", xt[0], XP, l, 0)
            for i in range(ntile):
                if i + 1 < ntile:
                    load_rows("sp", xt[(i + 1) % 3], XP, l, i + 1)
                layer_norm(xt[i % 3], jk, stt[i % 2], l2g, l2b, yt[i % 2])
                store_rows("sp", yt[i % 2], out if (last and i < 16) else X, l, i)
                yield

        def phase_mlstm(l, idx):
            last = l == DEPTH - 1
            W = ml_w_in[idx].rearrange("(k p) n -> p k n", p=128)
            P2 = Phase(fw)
            NCH = NT
            sel4 = P2.sb("sel4", [4, 4, 128], F32)
            COL = P2.sb("COL", [128, NCH, 32], F32)
            ACOL = P2.sb("ACOL", [128, 8, NCH], F32)
            BETA = [P2.sb("BETA%d" % d_, [4, T], F32) for d_ in range(2)]
            PR = Phase(fw)
            rows = {}
            for nm in ("I", "F"):
                for d_ in range(2):
                    rows[(nm, d_)] = PR.sb("row%s%d" % (nm, d_), [4, T], F32)
            ph = Phase(fw)
            hT = ph.sb("hT", [128, KC, T], BF16)
            hres = [Res("hT%d" % i) for i in range(NT)]
            phase_xt(l, hT, hres, 0, 16)
            wgt = ph.sb("wgt", [128, KC, 16], BF16)
            fw.dma("pool", (wgt[:], W[:, :, 6144:6160]), writes=[wgt], key=wgt.ds)
            gbt = ph.sb("gbt", [4, 4], F32)
            fw.dma("sp", (gbt[:], ml_gate_b[idx].rearrange("(a h) -> h a", h=4)), writes=[gbt], key=gbt.ds, allow_slow_non_contiguous=True)
            for (nm, d_, c0, a_) in (("I", 0, 0, 0), ("F", 0, 4, 1), ("I", 1, 8, 2), ("F", 1, 12, 3)):
                rw_ = rows[(nm, d_)]
                for gi, (t0, n) in enumerate(GROUPS5):
                    bank = pb[gi % 2]
                    for k in range(KC):
                        fw.op("pe", lambda g: g.matmul(bank[0:4, 0:n], lhsT=wgt[:, k, c0:c0 + 4], rhs=hT[:, k, t0:t0 + n], start=(k == 0), stop=(k == KC - 1)), reads=[wgt] + hres[t0 // 128:(t0 + n) // 128], writes=[bank])
                    fw.op("act", lambda g: g.activation(out=rw_[:, t0:t0 + n], in_=bank[0:4, 0:n], func=AF.Identity, bias=gbt[:, a_:a_ + 1], scale=1.0), reads=[bank, gbt], writes=[rw_])
            cvt = ph.sb("cvt", [48, 128], F32)
            cvT = ph.sb("cvT", [128, 48], F32)
            fw.dma("sp", (cvt[:], ml_conv[idx].rearrange("j (c p) -> (j c) p", p=128)), writes=[cvt], key=cvt.ds)
            fw.op("pe", lambda g: g.transpose(pb[2][:, 0:48], cvt[:], ident[0:48, 0:48]), reads=[cvt, ident], writes=[pb[2]])
            fw.op("dve", lambda g: g.tensor_copy(out=cvT[:], in_=pb[2][:, 0:48]), reads=[pb[2]], writes=[cvT])
            wb = [ph.sb("wb%d" % i, [128, KC, 512], BF16) for i in range(2)]
            praw = [ph.sb("praw%d" % i, [128, T + 4], F32) for i in range(1)] * 2
            acc = [ph.sb("acc%d" % i, [128, T], F32) for i in range(1)] * 2
            qo = [ph.sb("qo%d" % i, [128, T], BF16) for i in range(2)]
            for p_ in praw:
                fw.op("pool", lambda g: g.memset(p_[:], 0.0), writes=[p_])
            nb = 0
            nq = 0
            for blk in range(4):
                w = wb[blk % 2]
                fw.dma("pool", (w[:], W[:, :, blk * 512:(blk + 1) * 512]), writes=[w], key=w.ds)
                for c4 in range(4):
                    ch = blk * 4 + c4
                    p_ = praw[nq % 2]
                    a_ = acc[nq % 2]
                    o_ = qo[nq % 2]
                    nq += 1
                    for (t0, n) in GROUPS5:
                        bank = pb[nb % 6]
                        nb += 1
                        for k in range(KC):
                            fw.op("pe", lambda g: g.matmul(bank[:, 0:n], lhsT=w[:, k, c4 * 128:(c4 + 1) * 128], rhs=hT[:, k, t0:t0 + n], start=(k == 0), stop=(k == KC - 1)), reads=[w] + hres[t0 // 128:(t0 + n) // 128], writes=[bank])
                        off = 1 + t0 + (2 if t0 >= TL else 0)
                        fw.op("act", lambda g: g.copy(out=p_[:, off:off + n], in_=bank[:, 0:n]), reads=[bank], writes=[p_], skip_self=True)
                    for (s0, n, off) in ((0, TL, 1), (TL, TCX, TL + 3)):
                        fw.op("dve", lambda g: g.tensor_scalar(out=a_[:, s0:s0 + n], in0=p_[:, off:off + n], scalar1=cvT[:, 16 + ch:17 + ch], scalar2=None, op0=ALU.mult), reads=[p_, cvT], writes=[a_])
                        fw.op("dve", lambda g: g.scalar_tensor_tensor(out=a_[:, s0:s0 + n], in0=p_[:, off - 1:off - 1 + n], scalar=cvT[:, ch:ch + 1], in1=a_[:, s0:s0 + n], op0=ALU.mult, op1=ALU.add), reads=[p_, cvT, a_], writes=[a_])
                        fw.op("dve", lambda g: g.scalar_tensor_tensor(out=a_[:, s0:s0 + n], in0=p_[:, off + 1:off + 1 + n], scalar=cvT[:, 32 + ch:33 + ch], in1=a_[:, s0:s0 + n], op0=ALU.mult, op1=ALU.add), reads=[p_, cvT, a_], writes=[a_])
                    if ch < 8:
                        fw.op("act", lambda g: g.activation(out=o_[:], in_=a_[:], func=AF.Silu), reads=[a_], writes=[o_])
                    else:
                        fw.op("act", lambda g: g.activation(out=a_[:], in_=a_[:], func=AF.Silu), reads=[a_], writes=[a_])
                        fw.op("pool", lambda g: g.tensor_scalar(out=o_[:], in0=a_[:], scalar1=0.0625, scalar2=None, op0=ALU.mult), reads=[a_], writes=[o_])
                    fw.dma("sp", (QK[ch * 128:(ch + 1) * 128, :], o_[:]), reads=[o_], key=o_.ds)
            osb = [ph.sb("osb%d" % i, [128, 512], BF16) for i in range(3)]
            vsb = [ph.sb("vsb%d" % i, [128, 512], F32) for i in range(3)]
            no = 0
            for blk in range(4, 12):
                w = wb[blk % 2]
                fw.dma("pool", (w[:], W[:, :, blk * 512:(blk + 1) * 512]), writes=[w], key=w.ds)
                isv = blk < 8
                for i in range(NT if (isv or not last) else 16):
                    bank = pb[nb % 6]
                    nb += 1
                    for k in range(KC):
                        fw.op("pe", lambda g: g.matmul(bank[:], lhsT=hT[:, k, i * 128:(i + 1) * 128], rhs=w[:, k, :], start=(k == 0), stop=(k == KC - 1)), reads=[w, hres[i]], writes=[bank])
                    if isv:
                        o = osb[no % 3]
                        fw.op("act", lambda g: g.copy(out=o[:], in_=bank[:]), reads=[bank], writes=[o])
                        fw.dma("sp", (VS[i * 128:(i + 1) * 128, (blk - 4) * 512:(blk - 3) * 512], o[:]), reads=[o], key=o.ds)
                    else:
                        o = vsb[no % 3]
                        fw.op("act", lambda g: g.activation(out=o[:], in_=bank[:], func=AF.Sigmoid), reads=[bank], writes=[o])
                        fw.dma("sp", (V[i * 128:(i + 1) * 128, (blk - 8) * 512:(blk - 7) * 512], o[:]), reads=[o], key=o.ds)
                    no += 1
            ph.close()

            ph = Phase(fw)
            fw.op("pool", lambda g: g.memset(sel4[:], 0.0), writes=[sel4])
            fw.op("pool", lambda g: g.affine_select(out=sel4[:], in_=sel4[:], pattern=[[-1, 4], [0, 128]], compare_op=ALU.not_equal, fill=1.0, base=0, channel_multiplier=1), reads=[sel4], writes=[sel4])
            rmask = ph.sb("rmask", [4, T], F32)
            rneg = ph.sb("rneg", [4, T], F32)
            fw.op("pool", lambda g: g.memset(rmask[:], 1.0), writes=[rmask])
            fw.op("pool", lambda g: g.memset(rmask[:].rearrange("p (c t) -> p c t", t=128)[:, :, 0:1], 0.0), reads=[rmask], writes=[rmask])
            fw.op("pool", lambda g: g.memset(rneg[:], 0.0), writes=[rneg])
            fw.op("pool", lambda g: g.memset(rneg[:].rearrange("p (c t) -> p c t", t=128)[:, :, 0:1], -1e30), reads=[rneg], writes=[rneg])
            i4 = ident
            for d_ in range(2):
                I_, F_ = rows[("I", d_)], rows[("F", d_)]
                ph2 = ph
                ph = Phase(fw)
                B_ = ph.sb("B_%d" % d_, [4, T], F32)
                U_ = ph.sb("U_%d" % d_, [4, T], F32)
                CM = ph.sb("CM%d" % d_, [4, T], F32)
                TM = ph.sb("TM%d" % d_, [4, T], F32)
                DEC = ph.sb("DEC%d" % d_, [4, T], F32)
                ENG = ph.sb("ENG%d" % d_, [4, T], F32)
                KWS = ph.sb("KWS%d" % d_, [4, T], F32)
                BL = ph.sb("BL%d" % d_, [4, NCH], F32)
                UM = ph.sb("UM%d" % d_, [4, NCH], F32)
                MN = ph.sb("MN%d" % d_, [4, NCH], F32)
                MP = ph.sb("MP%d" % d_, [4, NCH], F32)
                AA = ph.sb("AA%d" % d_, [4, NCH], F32)
                rv = (lambda ap: ap) if d_ == 0 else (lambda ap: ap[:, ::-1])
                fw.op("act", lambda g: g.activation(out=F_[:], in_=F_[:], func=AF.Exp, scale=-1.0), reads=[F_], writes=[F_])
                fw.op("act", lambda g: g.activation(out=F_[:], in_=F_[:], func=AF.Ln, bias=1.0, scale=1.0), reads=[F_], writes=[F_])
                fw.op("dve", lambda g: g.tensor_scalar(out=F_[:], in0=F_[:], scalar1=-1.0, scalar2=None, op0=ALU.mult), reads=[F_], writes=[F_])
                for (s0, n) in ((0, TL), (TL, TCX)):
                    fw.op("dve", lambda g: g.tensor_tensor_scan(out=rv(B_[:, s0:s0 + n]), data0=rmask[:, 0:n], data1=rv(F_[:, s0:s0 + n]), initial=0.0, op0=ALU.mult, op1=ALU.add), reads=[F_, rmask], writes=[B_])
                fw.op("dve", lambda g: g.tensor_tensor(out=U_[:], in0=I_[:], in1=B_[:], op=ALU.subtract), reads=[I_, B_], writes=[U_])
                for (s0, n) in ((0, TL), (TL, TCX)):
                    fw.op("dve", lambda g: g.tensor_tensor_scan(out=rv(CM[:, s0:s0 + n]), data0=rneg[:, 0:n], data1=rv(U_[:, s0:s0 + n]), initial=-1e30, op0=ALU.add, op1=ALU.max), reads=[U_, rneg], writes=[CM])
                lastpos = 127 if d_ == 0 else 0
                b3 = B_[:].rearrange("p (c t) -> p c t", t=128)
                c3 = CM[:].rearrange("p (c t) -> p c t", t=128)
                fw.op("dve", lambda g: g.tensor_copy(out=BL[:], in_=b3[:, :, lastpos]), reads=[B_], writes=[BL])
                fw.op("dve", lambda g: g.tensor_copy(out=UM[:], in_=c3[:, :, lastpos]), reads=[CM], writes=[UM])
                if d_ == 0:
                    segs = [(16, 18, False), (0, 16, False)]
                else:
                    segs = [(16, 18, True), (0, 16, True)]
                init = 0.0
                for (a0, a1, rev) in segs:
                    sv = (lambda ap: ap[:, ::-1]) if rev else (lambda ap: ap)
                    ini = init
                    fw.op("dve", lambda g: g.tensor_tensor_scan(out=sv(MN[:, a0:a1]), data0=sv(UM[:, a0:a1]), data1=sv(BL[:, a0:a1]), initial=ini, op0=ALU.max, op1=ALU.add), reads=[UM, BL, MN], writes=[MN])
                    init = MN[:, (a0 if rev else a1 - 1):(a0 if rev else a1 - 1) + 1]
                if d_ == 0:
                    fw.op("dve", lambda g: g.memset(MP[:, 16:17], 0.0), writes=[MP])
                    fw.op("dve", lambda g: g.tensor_copy(out=MP[:, 17:18], in_=MN[:, 16:17]), reads=[MN], writes=[MP])
                    fw.op("dve", lambda g: g.tensor_copy(out=MP[:, 0:1], in_=MN[:, 17:18]), reads=[MN, MP], writes=[MP])
                    fw.op("dve", lambda g: g.tensor_copy(out=MP[:, 1:16], in_=MN[:, 0:15]), reads=[MN, MP], writes=[MP])
                else:
                    fw.op("dve", lambda g: g.memset(MP[:, 17:18], 0.0), writes=[MP])
                    fw.op("dve", lambda g: g.tensor_copy(out=MP[:, 16:17], in_=MN[:, 17:18]), reads=[MN], writes=[MP])
                    fw.op("dve", lambda g: g.tensor_copy(out=MP[:, 15:16], in_=MN[:, 16:17]), reads=[MN, MP], writes=[MP])
                    fw.op("dve", lambda g: g.tensor_copy(out=MP[:, 0:15], in_=MN[:, 1:16]), reads=[MN, MP], writes=[MP])
                mpb = MP[:].unsqueeze(2).to_broadcast([4, NCH, 128])
                t3 = TM[:].rearrange("p (c t) -> p c t", t=128)
                be3 = BETA[d_][:].rearrange("p (c t) -> p c t", t=128)
                fw.op("dve", lambda g: g.tensor_tensor(out=t3, in0=c3, in1=mpb, op=ALU.max), reads=[CM, MP], writes=[TM])
                fw.op("dve", lambda g: g.tensor_scalar(out=BETA[d_][:], in0=TM[:], scalar1=-1.0, scalar2=None, op0=ALU.mult), reads=[TM], writes=[BETA[d_]])
                fw.op("dve", lambda g: g.tensor_tensor(out=t3, in0=be3, in1=mpb, op=ALU.add), reads=[BETA[d_], MP, TM], writes=[TM])
                fw.op("act", lambda g: g.activation(out=DEC[:], in_=TM[:], func=AF.Exp), reads=[TM], writes=[DEC])
                fw.op("dve", lambda g: g.tensor_tensor(out=TM[:], in0=BETA[d_][:], in1=B_[:], op=ALU.subtract), reads=[BETA[d_], B_, DEC], writes=[TM])
                fw.op("act", lambda g: g.activation(out=ENG[:], in_=TM[:], func=AF.Exp), reads=[TM], writes=[ENG])
                fw.op("dve", lambda g: g.tensor_tensor(out=AA[:], in0=BL[:], in1=MN[:], op=ALU.subtract), reads=[BL, MN], writes=[AA])
                fw.op("dve", lambda g: g.tensor_tensor(out=t3, in0=U_[:].rearrange("p (c t) -> p c t", t=128), in1=AA[:].unsqueeze(2).to_broadcast([4, NCH, 128]), op=ALU.add), reads=[U_, AA, ENG], writes=[TM])
                fw.op("act", lambda g: g.activation(out=KWS[:], in_=TM[:], func=AF.Exp), reads=[TM], writes=[KWS])
                fw.op("dve", lambda g: g.tensor_tensor(out=AA[:], in0=AA[:], in1=MP[:], op=ALU.add), reads=[AA, MP], writes=[AA])
                fw.op("act", lambda g: g.activation(out=AA[:], in_=AA[:], func=AF.Exp), reads=[AA], writes=[AA])
                for c in range(NCH):
                    bank = pb[c % 2]
                    for qi, Q_ in enumerate((U_, DEC, ENG, KWS)):
                        fw.op("pe", lambda g: g.matmul(bank[:, qi * 4:qi * 4 + 4], lhsT=Q_[:, c * 128:(c + 1) * 128], rhs=i4[0:4, 0:4], start=True, stop=True), reads=[Q_, ident], writes=[bank])
                    fw.op("act", lambda g: g.copy(out=COL[:, c, :].rearrange("p (q e) -> p q e", e=8)[:, :, d_ * 4:d_ * 4 + 4], in_=bank[:, 0:16].rearrange("p (q e) -> p q e", e=4)), reads=[bank], writes=[COL])
                for h in range(4):
                    bank = pb[2 + h % 2]
                    fw.op("pe", lambda g: g.matmul(bank[:, 0:NCH], lhsT=sel4[:, h, :], rhs=AA[:], start=True, stop=True), reads=[sel4, AA], writes=[bank])
                    fw.op("act", lambda g: g.copy(out=ACOL[:, d_ * 4 + h, :], in_=bank[:, 0:NCH]), reads=[bank], writes=[ACOL])
                ph.close()
                ph = ph2
            ph.close()
            PR.close()

            ph = Phase(fw)
            ngB = ph.sb("ngB", [128, D], F32)
            fw.dma("sp", (ngB[:], ml_norm_g[idx].partition_broadcast(128)), writes=[ngB], key=ngB.ds)
            ntile_out = 16 if last else NT
            for h in range(4):
                hp = Phase(fw)
                qT = hp.sb("qT", [128, 2, T], BF16)
                kT = hp.sb("kT", [128, 2, T], BF16)
                vh = hp.sb("vh", [128, NT, 512], BF16)
                kt = hp.sb("kt", [128, NT, 256], BF16)
                hacc = hp.sb("hacc", [128, NT, 512], F32)
                hres_ = [Res("hacc%d" % i) for i in range(NT)]
                for dc in range(2):
                    fw.dma("sp", (qT[:, dc, :], QK[h * 256 + dc * 128:h * 256 + (dc + 1) * 128, :]), writes=[qT], key=qT.ds)
                    fw.dma("sp", (kT[:, dc, :], QK[1024 + h * 256 + dc * 128:1024 + h * 256 + (dc + 1) * 128, :]), writes=[kT], key=kT.ds)
                fw.dma("sp", (vh[:], VS[:, h * 512:(h + 1) * 512].rearrange("(c p) n -> p c n", p=128)), writes=[vh], key=vh.ds)
                for c in range(NT):
                    for dc in range(2):
                        fw.op("pe", lambda g: g.transpose(pbh[:, (c % 4) * 256 + dc * 128:(c % 4) * 256 + (dc + 1) * 128], kT[:, dc, c * 128:(c + 1) * 128], identb[:]), reads=[kT, identb], writes=[pbh])
                    fw.op("act", lambda g: g.copy(out=kt[:, c, :], in_=pbh[:, (c % 4) * 256:(c % 4 + 1) * 256]), reads=[pbh], writes=[kt])
                st = {}
                for d_ in range(2):
                    s = {}
                    s["C"] = hp.sb("C%d" % d_, [128, 2, 512], F32)
                    s["Cb"] = hp.sb("Cb%d" % d_, [128, 2, 512], BF16)
                    s["n"] = hp.sb("n%d" % d_, [128, 2], F32)
                    s["nb"] = hp.sb("nb%d" % d_, [128, 2], BF16)
                    s["ET"] = hp.sb("ET%d" % d_, [128, 128], F32)
                    s["ST"] = hp.sb("ST%d" % d_, [128, 128], BF16)
                    s["pd"] = hp.sb("pd%d" % d_, [128, 8], F32)
                    s["t1"] = hp.sb("t1%d" % d_, [128, 512], F32)
                    s["tm"] = hp.sb("tm%d" % d_, [128, 512], F32)
                    s["kw"] = hp.sb("kw%d" % d_, [128, 256], BF16)
                    s["bk"] = [pb[d_ * 3], pb[d_ * 3 + 1], pb[d_ * 3 + 2]]
                    for nm in ("C", "Cb", "n", "nb"):
                        b_ = s[nm]
                        fw.op("pool", lambda g: g.memset(b_[:], 0.0), writes=[b_])
                    st[d_] = s
                order = {0: [16, 17] + list(range(16)), 1: [17, 16] + list(range(15, -1, -1))}
                hwritten = set()
                for step in range(NT):
                    for d_ in range(2):
                        s = st[d_]
                        c = order[d_][step]
                        tk = slice(c * 128, (c + 1) * 128)
                        bA, bB, bC = s["bk"]
                        colu = COL[:, c, 0 * 8 + d_ * 4 + h:0 * 8 + d_ * 4 + h + 1]
                        cold = COL[:, c, 1 * 8 + d_ * 4 + h:1 * 8 + d_ * 4 + h + 1]
                        cole = COL[:, c, 2 * 8 + d_ * 4 + h:2 * 8 + d_ * 4 + h + 1]
                        colk = COL[:, c, 3 * 8 + d_ * 4 + h:3 * 8 + d_ * 4 + h + 1]
                        cola = ACOL[:, d_ * 4 + h, c:c + 1]
                        msk = masklo if d_ == 0 else maskhi
                        fw.op("pe", lambda g: g.matmul(bA[:, 0:128], lhsT=sel4[:, h, :], rhs=BETA[d_][:, tk], start=True, stop=True), reads=[sel4, BETA[d_]], writes=[bA])
                        for dc in range(2):
                            fw.op("pe", lambda g: g.matmul(bA[:, 128:256], lhsT=kT[:, dc, tk], rhs=qT[:, dc, tk], start=(dc == 0), stop=(dc == 1)), reads=[kT, qT], writes=[bA])
                        fw.op("act", lambda g: g.activation(out=s["ET"][:], in_=bA[:, 0:128], func=AF.Exp, bias=colu, scale=1.0), reads=[bA, COL], writes=[s["ET"]])
                        fw.op("pool", lambda g: g.tensor_tensor(out=s["ET"][:], in0=s["ET"][:], in1=msk[:], op=ALU.mult), reads=[s["ET"], msk], writes=[s["ET"]])
                        fw.op("dve", lambda g: g.tensor_tensor(out=s["ST"][:], in0=bA[:, 128:256], in1=s["ET"][:], op=ALU.mult), reads=[bA, s["ET"]], writes=[s["ST"]])
                        for dc in range(2):
                            fw.op("pe", lambda g: g.matmul(bA[:, 256:257], lhsT=qT[:, dc, tk], rhs=s["nb"][:, dc:dc + 1], start=(dc == 0), stop=(dc == 1)), reads=[qT, s["nb"]], writes=[bA])
                        fw.op("pe", lambda g: g.matmul(bA[:, 257:258], lhsT=s["ST"][:], rhs=onesb[:, 0:1], start=True, stop=True), reads=[s["ST"], onesb], writes=[bA])
                        for dc in range(2):
                            fw.op("pe", lambda g: g.matmul(bB[:], lhsT=qT[:, dc, tk], rhs=s["Cb"][:, dc, :], start=(dc == 0), stop=(dc == 1)), reads=[qT, s["Cb"]], writes=[bB])
                        fw.op("pe", lambda g: g.matmul(bC[:], lhsT=s["ST"][:], rhs=vh[:, c, :], start=True, stop=True), reads=[s["ST"], vh], writes=[bC])
                        pd = s["pd"]
                        fw.op("act", lambda g: g.copy(out=pd[:, 0:2], in_=bA[:, 256:258]), reads=[bA], writes=[pd])
                        fw.op("dve", lambda g: g.scalar_tensor_tensor(out=pd[:, 2:3], in0=pd[:, 0:1], scalar=cold, in1=pd[:, 1:2], op0=ALU.mult, op1=ALU.add), reads=[pd, COL], writes=[pd])
                        fw.op("dve", lambda g: g.tensor_scalar(out=pd[:, 3:4], in0=pd[:, 2:3], scalar1=-1.0, scalar2=None, op0=ALU.mult), reads=[pd], writes=[pd])
                        fw.op("dve", lambda g: g.tensor_tensor(out=pd[:, 3:4], in0=pd[:, 3:4], in1=pd[:, 2:3], op=ALU.max), reads=[pd], writes=[pd])
                        fw.op("dve", lambda g: g.tensor_tensor(out=pd[:, 4:5], in0=pd[:, 3:4], in1=cole, op=ALU.max), reads=[pd, COL], writes=[pd])
                        fw.op("dve", lambda g: g.reciprocal(out=pd[:, 5:6], in_=pd[:, 4:5]), reads=[pd], writes=[pd])
                        fw.op("dve", lambda g: g.tensor_tensor(out=pd[:, 6:7], in0=pd[:, 5:6], in1=cold, op=ALU.mult), reads=[pd, COL], writes=[pd])
                        fw.op("act", lambda g: g.activation(out=s["t1"][:], in_=bB[:], func=AF.Copy, scale=pd[:, 6:7]), reads=[bB, pd], writes=[s["t1"]])
                        if c not in hwritten:
                            hwritten.add(c)
                            fw.op("dve", lambda g: g.scalar_tensor_tensor(out=hacc[:, c, :], in0=bC[:], scalar=pd[:, 5:6], in1=s["t1"][:], op0=ALU.mult, op1=ALU.add), reads=[bC, pd, s["t1"]], writes=[hres_[c]])
                        else:
                            fw.op("dve", lambda g: g.scalar_tensor_tensor(out=s["tm"][:], in0=bC[:], scalar=pd[:, 5:6], in1=s["t1"][:], op0=ALU.mult, op1=ALU.add), reads=[bC, pd, s["t1"]], writes=[s["tm"]])
                            fw.op("pool", lambda g: g.tensor_tensor(out=hacc[:, c, :], in0=hacc[:, c, :], in1=s["tm"][:], op=ALU.add), reads=[s["tm"], hres_[c]], writes=[hres_[c]])
                        if step < NT - 1:
                            fw.op("pool", lambda g: g.tensor_scalar(out=s["kw"][:], in0=kt[:, c, :], scalar1=colk, scalar2=None, op0=ALU.mult), reads=[kt, COL], writes=[s["kw"]])
                            for dc, bk_ in ((0, bB), (1, bC)):
                                fw.op("pe", lambda g: g.matmul(bk_[:], lhsT=s["kw"][:, dc * 128:(dc + 1) * 128], rhs=vh[:, c, :], start=True, stop=True), reads=[s["kw"], vh], writes=[bk_])
                                fw.op("pe", lambda g: g.matmul(bA[:, 260 + dc:261 + dc], lhsT=s["kw"][:, dc * 128:(dc + 1) * 128], rhs=onesb[:, 0:1], start=True, stop=True), reads=[s["kw"], onesb], writes=[bA])
                            for dc, bk_ in ((0, bB), (1, bC)):
                                fw.op("dve", lambda g: g.scalar_tensor_tensor(out=s["C"][:, dc, :], in0=s["C"][:, dc, :], scalar=cola, in1=bk_[:], op0=ALU.mult, op1=ALU.add), reads=[s["C"], ACOL, bk_], writes=[s["C"]])
                            fw.op("act", lambda g: g.copy(out=s["Cb"][:], in_=s["C"][:]), reads=[s["C"]], writes=[s["Cb"]])
                            fw.op("dve", lambda g: g.scalar_tensor_tensor(out=s["n"][:], in0=s["n"][:], scalar=cola, in1=bA[:, 260:262], op0=ALU.mult, op1=ALU.add), reads=[s["n"], ACOL, bA], writes=[s["n"]])
                            fw.op("act", lambda g: g.copy(out=s["nb"][:], in_=s["n"][:]), reads=[s["n"]], writes=[s["nb"]])
                og = [hp.sb("og%d" % i, [128, 512], F32) for i in range(2)]
                zo = [hp.sb("zo%d" % i, [128, 512], BF16) for i in range(2)]
                jk = hp.sb("jk", [128, 512], BF16)
                ss = [hp.sb("ss%d" % i, [128, 8], F32) for i in range(2)]
                for i in range(ntile_out):
                    o_, z_, s_ = og[i % 2], zo[i % 2], ss[i % 2]
                    fw.dma("sp", (o_[:], V[i * 128:(i + 1) * 128, h * 512:(h + 1) * 512]), writes=[o_], key=o_.ds)
                    hv = hacc[:, i, :]
                    fw.op("act", lambda g: g.activation(out=jk[:], in_=hv, func=AF.Square, accum_out=s_[:, 0:1]), reads=[hres_[i]], writes=[jk, s_])
                    fw.op("dve", lambda g: g.tensor_scalar(out=s_[:, 1:2], in0=s_[:, 0:1], scalar1=1.0 / 512, scalar2=RMS_EPS, op0=ALU.mult, op1=ALU.add), reads=[s_], writes=[s_])
                    fw.op("act", lambda g: g.activation(out=s_[:, 2:3], in_=s_[:, 1:2], func=AF.Sqrt), reads=[s_], writes=[s_])
                    fw.op("dve", lambda g: g.reciprocal(out=s_[:, 3:4], in_=s_[:, 2:3]), reads=[s_], writes=[s_])
                    fw.op("dve", lambda g: g.scalar_tensor_tensor(out=hv, in0=hv, scalar=s_[:, 3:4], in1=ngB[:, h * 512:(h + 1) * 512], op0=ALU.mult, op1=ALU.mult), reads=[hres_[i], s_, ngB], writes=[hres_[i]])
                    fw.op("pool", lambda g: g.tensor_tensor(out=z_[:], in0=hv, in1=o_[:], op=ALU.mult), reads=[hres_[i], o_], writes=[z_])
                    fw.dma("sp", (ZS[i * 128:(i + 1) * 128, h * 512:(h + 1) * 512], z_[:]), reads=[z_], key=z_.ds)
                hp.close()
            ph.close()
            P2.close()

            ph = Phase(fw)
            gB = [ph.sb("gB%d" % i, [128, D], F32) for i in range(2)]
            build_gb(gB, 32)
            wo = ph.sb("wo", [128, KC, D], BF16)
            wov = ml_w_out[idx].rearrange("(k p) n -> p k n", p=128)
            woh = [Res("wo%d" % q) for q in range(4)]
            for q in range(4):
                fw.dma("pool", (wo[:, :, q * 512:(q + 1) * 512], wov[:, :, q * 512:(q + 1) * 512]), writes=[woh[q]], key=fw.get_dsem())
            l1g = ph.sb("l1g", [128, D], F32)
            l1b = ph.sb("l1b", [128, D], F32)
            fw.dma("sp", (l1g[:], ln1_g[l].partition_broadcast(128)), writes=[l1g], key=l1g.ds)
            fw.dma("sp", (l1b[:], ln1_b[l].partition_broadcast(128)), writes=[l1b], key=l1b.ds)
            zt = [ph.sb("zt%d" % i, [128, D], BF16) for i in range(2)]
            zT = [ph.sb("zT%d" % i, [128, KC, 128], BF16) for i in range(2)]
            xt = [ph.sb("xt%d" % i, [128, D], F32) for i in range(2)]
            yt = [ph.sb("yt%d" % i, [128, D], F32) for i in range(2)]
            jk = ph.sb("jk", [128, D], BF16)
            st2 = [ph.sb("stb%d" % i, [128, 8], F32) for i in range(2)]

            def loads5(i):
                fw.dma("sp", (zt[i % 2][:], ZS[i * 128:(i + 1) * 128, :]), writes=[zt[i % 2]], key=zt[i % 2].ds)
                load_rows("sp", xt[i % 2], X, l, i)
            loads5(0)
            nb = 0
            for i in range(ntile_out):
                if i + 1 < ntile_out:
                    loads5(i + 1)
                z_, zT_, x_, y_ = zt[i % 2], zT[i % 2], xt[i % 2], yt[i % 2]
                for q in range(2):
                    for cc in range(8):
                        c = q * 8 + cc
                        fw.op("pe", lambda g: g.transpose(pbh[:, cc * 128:(cc + 1) * 128], z_[:, c * 128:(c + 1) * 128], identb[:]), reads=[z_, identb], writes=[pbh])
                    fw.op("act", lambda g: g.copy(out=zT_[:, q * 8:(q + 1) * 8, :].rearrange("p c n -> p (c n)"), in_=pbh[:]), reads=[pbh], writes=[zT_])
                yb = []
                for q in range(4):
                    bank = pb[nb % 7]
                    nb += 1
                    for k in range(KC):
                        fw.op("pe", lambda g: g.matmul(bank[:], lhsT=zT_[:, k, :], rhs=wo[:, k, q * 512:(q + 1) * 512], start=(k == 0), stop=(k == KC - 1)), reads=[zT_, woh[q]], writes=[bank])
                    yb.append(bank)
                resid_ln(gB, (x_, y_, jk, st2[i % 2]), l, i, yb, l1g, l1b, X)
            ph.close()

        phase_ada(0)
        for l in range(nlayers):
            if stop == ("ada", l):
                break
            if l % 2 == 0:
                phase_gmlp(l, l // 2)
            else:
                phase_mlstm(l, l // 2)
            if stop == ("mix", l):
                break
            phase_moe(l)
        fw.barrier()
        G.close()
    return nc


_CACHE = {}


def kernel(**inputs):
    inp = {k: np.ascontiguousarray(np.asarray(v, dtype=np.float32)) for k, v in inputs.items()}
    if "nc" not in _CACHE:
        _CACHE["nc"] = build_program()
    nc = _CACHE["nc"]
    shared = {k: v for k, v in inp.items() if k not in ("x", "c", "ctx", "c_ctx")}
    shared["router_b"] = inp["router_b"].reshape(1, NEXP)
    in_maps = []
    for core in range(8):
        b = core % 4
        m = dict(shared)
        m["x"] = np.ascontiguousarray(inp["x"][b])
        m["c"] = np.ascontiguousarray(inp["c"][b:b + 1])
        m["ctx"] = np.ascontiguousarray(inp["ctx"][b])
        m["c_ctx"] = inp["c_ctx"].reshape(1, D)
        in_maps.append(m)
    res = run_bass_kernel_spmd(nc, in_maps, core_ids=list(range(8)))
    return np.stack([np.asarray(res.results[b]["out"], dtype=np.float32) for b in range(4)], axis=0)
```

```python
import numpy as np
from contextlib import ExitStack
import concourse.bass as bass
import concourse.mybir as mybir
from concourse.bass_utils import run_bass_kernel_spmd

F32 = mybir.dt.float32
BF16 = mybir.dt.bfloat16
AF = mybir.ActivationFunctionType
ALU = mybir.AluOpType
AX = mybir.AxisListType

D = 2048
KC = 16
TL = 2048
TCX = 256
T = TL + TCX
NT = T // 128
DEPTH = 4
ALPHA = float((2 * DEPTH) ** 0.25)
LN_EPS = 1e-5
RMS_EPS = 1e-6
NEXP = 16
DEXP = 512
GROUPS5 = [(0, 512), (512, 512), (1024, 512), (1536, 512), (2048, 256)]


class Res:
    __slots__ = ("name", "w", "rs", "excl")

    def __init__(self, name):
        self.name = name
        self.w = None
        self.rs = {}
        self.excl = False


class Buf:
    def __init__(self, fw, t, name):
        self.fw = fw
        self.t = t
        self.r = Res(name)
        self._ds = None

    @property
    def ds(self):
        if self._ds is None:
            self._ds = self.fw.get_dsem()
        return self._ds

    def __getitem__(self, k):
        return self.t[k]


class FW:
    def __init__(self, nc, es):
        self.nc = nc
        self.es = es
        self.eng = {"pe": nc.tensor, "act": nc.scalar, "dve": nc.vector, "pool": nc.gpsimd, "sp": nc.sync}
        self.semobj = {}
        self.cnt = {}
        for k in ("pe", "act", "dve", "pool"):
            self.semobj[k] = es.enter_context(nc.semaphore("s_" + k))
            self.cnt[k] = 0
        self.seen = {k: {} for k in self.eng}
        self.dpool = []
        self.swmap = {}
        self.dn = 0
        self.uid = 0
        self.live_ds = []

    def get_dsem(self):
        if self.dpool:
            k = self.dpool.pop()
        else:
            self.dn += 1
            k = "d%d" % self.dn
            self.semobj[k] = self.es.enter_context(self.nc.semaphore("s_" + k))
            self.cnt[k] = 0
        self.live_ds.append(k)
        return k

    def _waits(self, e, reads, writes, skip_self=False):
        deps = {}
        for r in reads:
            if r.w is not None:
                k, v = r.w
                if deps.get(k, 0) < v:
                    deps[k] = v
            if r.excl:
                for k, v in r.rs.items():
                    if k != e and deps.get(k, 0) < v:
                        deps[k] = v
        for w in writes:
            if w.w is not None:
                k, v = w.w
                if deps.get(k, 0) < v:
                    deps[k] = v
            for k, v in w.rs.items():
                if deps.get(k, 0) < v:
                    deps[k] = v
        seen = self.seen[e]
        for k, v in deps.items():
            if k == e and (e == "pe" or skip_self):
                continue
            if seen.get(k, 0) >= v:
                continue
            self.eng[e].wait_ge(self.semobj[k], v)
            seen[k] = v

    def _done(self, comp, reads, writes):
        k, v = comp
        for r in reads:
            if r.rs.get(k, 0) < v:
                r.rs[k] = v
        for w in writes:
            w.w = comp
            w.rs = {}

    def op(self, e, fn, reads=(), writes=(), skip_self=False):
        reads = [x.r if isinstance(x, Buf) else x for x in reads]
        writes = [x.r if isinstance(x, Buf) else x for x in writes]
        self._waits(e, reads, writes, skip_self)
        ins = fn(self.eng[e])
        self.cnt[e] += 1
        ins.then_inc(self.semobj[e], 1)
        self._done((e, self.cnt[e]), reads, writes)

    def dma(self, e, pairs, reads=(), writes=(), key=None, **kw):
        reads = [x.r if isinstance(x, Buf) else x for x in reads]
        writes = [x.r if isinstance(x, Buf) else x for x in writes]
        if not isinstance(pairs, list):
            pairs = [pairs]
        if e == "pool":
            if key not in self.swmap:
                self.dn += 1
                k2 = "w%d" % self.dn
                self.semobj[k2] = self.es.enter_context(self.nc.semaphore("s_" + k2))
                self.cnt[k2] = 0
                self.swmap[key] = k2
            key = self.swmap[key]
        self._waits(e, reads, writes)
        for (o, i) in pairs:
            ins = self.eng[e].dma_start(out=o, in_=i, **kw)
            self.cnt[key] += 16
            ins.then_inc(self.semobj[key], 16)
        self._done((key, self.cnt[key]), reads, writes)

    def barrier(self):
        for e in self.eng:
            seen = self.seen[e]
            for k, v in self.cnt.items():
                if v > seen.get(k, 0):
                    self.eng[e].wait_ge(self.semobj[k], v)
                    seen[k] = v


class Phase:
    def __init__(self, fw):
        self.fw = fw
        self.es = ExitStack()
        self.mark = len(fw.live_ds)

    def sb(self, name, shape, dt):
        self.fw.uid += 1
        t = self.es.enter_context(self.fw.nc.sbuf_tensor("%s_%d" % (name, self.fw.uid), shape, dt))
        return Buf(self.fw, t, name)

    def close(self):
        fw = self.fw
        fw.barrier()
        self.es.close()
        rel = fw.live_ds[self.mark:]
        del fw.live_ds[self.mark:]
        fw.dpool.extend(rel)


def build_program(nlayers=DEPTH, debug=False, stop=None):
    nc = bass.Bass("TRN2", target_bir_lowering=False)

    def din(name, shape):
        return nc.dram_tensor(name, list(shape), F32, kind="ExternalInput").ap()

    x_in = din("x", (TL, D))
    c_in = din("c", (1, D))
    ctx_in = din("ctx", (TCX, D))
    cctx_in = din("c_ctx", (1, D))
    ada_w = din("ada_w", (DEPTH, D, 6 * D))
    ada_b = din("ada_b", (DEPTH, 6 * D))
    gm_w_in = din("gm_w_in", (2, D, 2 * D))
    gm_ln_g = din("gm_ln_g", (2, D))
    gm_ln_b = din("gm_ln_b", (2, D))
    gm_ws = din("gm_ws", (2, 8, 128, 128))
    gm_bs = din("gm_bs", (2, 8, 128))
    gm_w_out = din("gm_w_out", (2, D, D))
    ml_w_in = din("ml_w_in", (2, D, 6160))
    ml_conv = din("ml_conv", (2, 3, D))
    ml_gate_b = din("ml_gate_b", (2, 16))
    ml_norm_g = din("ml_norm_g", (2, D))
    ml_w_out = din("ml_w_out", (2, D, D))
    ln1_g = din("ln1_g", (DEPTH, D))
    ln1_b = din("ln1_b", (DEPTH, D))
    ln2_g = din("ln2_g", (DEPTH, D))
    ln2_b = din("ln2_b", (DEPTH, D))
    router_w = din("router_w", (D, NEXP))
    router_b = din("router_b", (1, NEXP))
    ex_w_gate = din("ex_w_gate", (DEPTH, NEXP, D, DEXP))
    ex_w_up = din("ex_w_up", (DEPTH, NEXP, D, DEXP))
    ex_w_down = din("ex_w_down", (DEPTH, NEXP, DEXP, D))
    out = nc.dram_tensor("out", [TL, D], F32, kind="ExternalOutput").ap()

    def scratch(name, shape, dt):
        return nc.dram_tensor(name, list(shape), dt, kind="ExternalOutput" if (debug and name in ("X", "V", "UTs", "XP", "QK", "VS", "ZS")) else "Internal").ap()

    X = scratch("X", (T, D), F32)
    XP = scratch("XP", (T, D), F32)
    V = scratch("V", (T, D), F32)
    UTs = scratch("UTs", (NT, 128, KC * 128), BF16)
    HTs = scratch("HTs", (NT, 128, 64 * 128), BF16)
    QK = scratch("QK", (2048, T), BF16)
    VS = scratch("VS", (T, D), BF16)
    ZS = scratch("ZS", (T, D), BF16)

    with ExitStack() as es:
        fw = FW(nc, es)
        G = Phase(fw)
        pb = []
        for i in range(7):
            pb.append(Buf(fw, es.enter_context(nc.psum_tensor("pb%d" % i, [128, 512], F32)), "pb%d" % i))
        pbh = Buf(fw, es.enter_context(nc.psum_tensor("pbh", [128, 1024], BF16)), "pbh")
        for b_ in pb + [pbh]:
            b_.r.excl = True

        ident = G.sb("ident", [128, 128], F32)
        identb = G.sb("identb", [128, 128], BF16)
        ones32 = G.sb("ones32", [128, 128], F32)
        onesb = G.sb("onesb", [128, 2], BF16)
        condT = G.sb("condT", [128, KC, 2], F32)
        modT = G.sb("modT", [128, 96, 2], F32)
        masklo = G.sb("masklo", [128, 128], F32)
        maskhi = G.sb("maskhi", [128, 128], F32)

        fw.op("pool", lambda g: g.memset(ident[:], 0.0), writes=[ident])
        fw.op("pool", lambda g: g.affine_select(out=ident[:], in_=ident[:], pattern=[[-1, 128]], compare_op=ALU.not_equal, fill=1.0, base=0, channel_multiplier=1), reads=[ident], writes=[ident])
        fw.op("dve", lambda g: g.tensor_copy(out=identb[:], in_=ident[:]), reads=[ident], writes=[identb])
        fw.op("pool", lambda g: g.memset(ones32[:], 1.0), writes=[ones32])
        fw.op("pool", lambda g: g.memset(onesb[:], 1.0), writes=[onesb])
        fw.op("pool", lambda g: g.memset(masklo[:], 1.0), writes=[masklo])
        fw.op("pool", lambda g: g.affine_select(out=masklo[:], in_=masklo[:], pattern=[[1, 128]], compare_op=ALU.is_ge, fill=0.0, base=0, channel_multiplier=-1), reads=[masklo], writes=[masklo])
        fw.op("pool", lambda g: g.memset(maskhi[:], 1.0), writes=[maskhi])
        fw.op("pool", lambda g: g.affine_select(out=maskhi[:], in_=maskhi[:], pattern=[[-1, 128]], compare_op=ALU.is_ge, fill=0.0, base=0, channel_multiplier=1), reads=[maskhi], writes=[maskhi])

        ph = Phase(fw)
        csb = ph.sb("csb", [16, 2, 128], F32)
        fw.dma("sp", (csb[:, 0, :], c_in.rearrange("o (k p) -> (o k) p", p=128)), writes=[csb], key=csb.ds)
        fw.dma("sp", (csb[:, 1, :], cctx_in.rearrange("o (k p) -> (o k) p", p=128)), writes=[csb], key=csb.ds)
        fw.op("act", lambda g: g.activation(out=csb[:], in_=csb[:], func=AF.Silu), reads=[csb], writes=[csb])
        for r in range(2):
            fw.op("pe", lambda g: g.transpose(pb[0][:, r * 16:(r + 1) * 16], csb[:, r, :], ident[0:16, 0:16]), reads=[csb, ident], writes=[pb[0]])
        for r in range(2):
            fw.op("dve", lambda g: g.tensor_copy(out=condT[:, :, r], in_=pb[0][:, r * 16:(r + 1) * 16]), reads=[pb[0]], writes=[condT])
        cpk = fw.get_dsem()
        xres = Res("Xdram")
        fw.dma("sp", [(X[0:TL, :], x_in[:, :]), (X[TL:T, :], ctx_in[:, :])], writes=[xres], key=cpk)
        ph.close()

        def xrows(A, l, i, c0=0, c1=D):
            if i >= 16 or l != 3:
                return [(slice(0, 128), A[i * 128:(i + 1) * 128, c0:c1])]
            v = A[0:TL, c0:c1].rearrange("(r w) d -> w r d", w=64)
            return [(slice(wl * 32, (wl + 1) * 32), v[i * 4 + wl]) for wl in range(4)]

        def load_rows(e, buf, A, l, i, c0=0, c1=D, ap=None):
            dst = buf.t if ap is None else ap
            fw.dma(e, [(dst[ps, :] if ap is None else ap[ps], src) for ps, src in xrows(A, l, i, c0, c1)], writes=[buf], key=buf.ds)

        def store_rows(e, buf, A, l, i, c0=0, c1=D, srcap=None):
            fw.dma(e, [(dst, (buf.t if srcap is None else srcap)[ps]) for ps, dst in xrows(A, l, i, c0, c1)], reads=[buf], key=buf.ds)

        def phase_ada(l):
            ph = Phase(fw)
            for _ in gen_ada(l, ph):
                pass
            ph.close()

        def gen_ada(l, ph):
            abt = ph.sb("abt", [96, 128], F32)
            abT = ph.sb("abT", [128, 96], F32)
            aw = [ph.sb("aw%d" % i, [128, KC, 512], BF16) for i in range(3)]
            condTb = ph.sb("condTb", [128, KC, 2], BF16)
            fw.op("dve", lambda g: g.tensor_copy(out=condTb[:], in_=condT[:]), reads=[condT], writes=[condTb])
            fw.dma("sp", (abt[:], ada_b[l].rearrange("(c p) -> c p", p=128)), writes=[abt], key=abt.ds)
            fw.op("pe", lambda g: g.transpose(pb[1][:, 0:96], abt[:], ident[0:96, 0:96]), reads=[abt, ident], writes=[pb[1]])
            fw.op("dve", lambda g: g.tensor_copy(out=abT[:], in_=pb[1][:, 0:96]), reads=[pb[1]], writes=[abT])
            awv = ada_w[l].rearrange("(k p) n -> p k n", p=128)
            for j in range(24):
                a = aw[j % 3]
                fw.dma("pool", (a[:], awv[:, :, j * 512:(j + 1) * 512]), writes=[a], key=a.ds)
                for c2 in range(4):
                    cidx = j * 4 + c2
                    for k in range(KC):
                        fw.op("pe", lambda g: g.matmul(pb[0][:, 2 * cidx:2 * cidx + 2], lhsT=a[:, k, c2 * 128:(c2 + 1) * 128], rhs=condTb[:, k, :], start=(k == 0), stop=(k == KC - 1)), reads=[a, condTb], writes=[pb[0]])
                yield
            pv = pb[0][:, 0:192].rearrange("p (c r) -> p c r", r=2)
            for r in range(2):
                fw.op("dve", lambda g: g.tensor_tensor(out=modT[:, :, r], in0=pv[:, :, r], in1=abT[:], op=ALU.add), reads=[pb[0], abT], writes=[modT])
            for c0 in (16, 64):
                fw.op("dve", lambda g: g.tensor_scalar_add(out=modT[:, c0:c0 + 16, :], in0=modT[:, c0:c0 + 16, :], scalar1=1.0), reads=[modT], writes=[modT])

        def build_gb(gB, cidx0):
            ph = Phase(fw)
            dg = [ph.sb("dg%d" % i, [128, 128], F32) for i in range(2)]
            n = 0
            for r in range(2):
                for q in range(4):
                    bank = pb[1 + (n % 2)]
                    n += 1
                    for cc in range(4):
                        c = q * 4 + cc
                        d_ = dg[c % 2]
                        fw.op("dve", lambda g: g.tensor_scalar(out=d_[:], in0=ident[:], scalar1=modT[:, cidx0 + c, r:r + 1], scalar2=None, op0=ALU.mult), reads=[ident, modT], writes=[d_])
                        fw.op("pe", lambda g: g.matmul(bank[:, cc * 128:(cc + 1) * 128], lhsT=ones32[:], rhs=d_[:], start=True, stop=True), reads=[ones32, d_], writes=[bank])
                    fw.op("act", lambda g: g.copy(out=gB[r][:, q * 512:(q + 1) * 512], in_=bank[:]), reads=[bank], writes=[gB[r]])
            ph.close()

        def phase_xt(l, hT, hres, sh0, sc0, router=None):
            ph = Phase(fw)
            xt = [ph.sb("xt%d" % i, [128, D], F32) for i in range(3)]
            if router is not None:
                rw = ph.sb("rw", [128, KC, NEXP], F32)
                rbB = ph.sb("rbB", [128, NEXP], F32)
                xm32 = [ph.sb("xm32_%d" % i, [128, KC, 128], F32) for i in range(2)]
                gs = {n_: [ph.sb("%s%d" % (n_, i), s_, F32) for i in range(2)] for n_, s_ in (("lg", [128, 16]), ("ee", [128, 16]), ("tt", [128, 16]), ("gt", [128, 16]), ("m1", [128, 4]), ("m2", [128, 4]), ("sc", [128, 4]), ("gm", [128, 4]), ("c1", [128, 4]))}
                import os as _os
                XB = int(_os.environ.get("XB", "0"))
                if not (XB & 2):
                    fw.dma("sp", (rw[:], router_w.rearrange("(k p) e -> p k e", p=128)), writes=[rw], key=rw.ds)
                    fw.dma("sp", (rbB[:], router_b[0].partition_broadcast(128)), writes=[rbB], key=rbB.ds)
                gateT = router
            ntile = NT
            if router is not None and (XB & 4):
                ntile = 0
            if ntile > 0:
                load_rows("sp", xt[0], X, l, 0)
            for i in range(ntile):
                r = 0 if i < 16 else 1
                if i + 1 < ntile:
                    load_rows("sp", xt[(i + 1) % 3], X, l, i + 1)
                x_ = xt[i % 3]
                ev = "act" if i % 2 == 0 else "dve"
                for q in range(4):
                    bank = pb[(i % 2) * 2 + (q % 2)]
                    for cc in range(4):
                        c = q * 4 + cc
                        fw.op("pe", lambda g: g.transpose(bank[:, cc * 128:(cc + 1) * 128], x_[:, c * 128:(c + 1) * 128], ident[:]), reads=[x_, ident], writes=[bank])
                    for cc in range(4):
                        c = q * 4 + cc
                        src = bank[:, cc * 128:(cc + 1) * 128]
                        dst = hT[:, c, i * 128:(i + 1) * 128]
                        scp = modT[:, sc0 + c, r:r + 1]
                        shp = modT[:, sh0 + c, r:r + 1]
                        if ev == "act":
                            fw.op("act", lambda g: g.activation(out=dst, in_=src, func=AF.Identity, bias=shp, scale=scp), reads=[bank, modT], writes=[hres[i]], skip_self=True)
                        else:
                            fw.op("dve", lambda g: g.tensor_scalar(out=dst, in0=src, scalar1=scp, scalar2=shp, op0=ALU.mult, op1=ALU.add), reads=[bank, modT], writes=[hres[i]], skip_self=True)
                        if router is not None:
                            xm = xm32[i % 2]
                            ev2 = "dve" if ev == "act" else "act"
                            d2 = xm[:, c, :]
                            if ev2 == "act":
                                fw.op("act", lambda g: g.activation(out=d2, in_=src, func=AF.Identity, bias=shp, scale=scp), reads=[bank, modT], writes=[xm], skip_self=True)
                            else:
                                fw.op("dve", lambda g: g.tensor_scalar(out=d2, in0=src, scalar1=scp, scalar2=shp, op0=ALU.mult, op1=ALU.add), reads=[bank, modT], writes=[xm], skip_self=True)
                import os as _os
                RM = int(_os.environ.get("RM", "3"))
                if router is not None and RM >= 1:
                    xm = xm32[i % 2]
                    bk = pb[4 + (i % 2)]
                    for k in range(KC):
                        fw.op("pe", lambda g: g.matmul(bk[:, 0:16], lhsT=xm[:, k, :], rhs=rw[:, k, :], start=(k == 0), stop=(k == KC - 1)), reads=[xm, rw], writes=[bk])
                    s = {n_: v_[i % 2] for n_, v_ in gs.items()}
                    V_ = "dve"
                    fw.op(V_, lambda g: g.tensor_tensor(out=s["lg"][:], in0=bk[:, 0:16], in1=rbB[:], op=ALU.add), reads=[bk, rbB], writes=[s["lg"]])
                    if RM < 2:
                        continue
                    fw.op(V_, lambda g: g.reduce_max(out=s["c1"][:, 0:1], in_=s["lg"][:], axis=AX.X), reads=[s["lg"]], writes=[s["c1"]])
                    fw.op(V_, lambda g: g.tensor_scalar(out=s["c1"][:, 1:2], in0=s["c1"][:, 0:1], scalar1=-1.0, scalar2=None, op0=ALU.mult), reads=[s["c1"]], writes=[s["c1"]])
                    fw.op("act", lambda g: g.activation(out=s["ee"][:], in_=s["lg"][:], func=AF.Exp, bias=s["c1"][:, 1:2], scale=1.0), reads=[s["lg"], s["c1"]], writes=[s["ee"]])
                    e3 = s["ee"][:].rearrange("p (g e) -> p g e", e=4)
                    t3 = s["tt"][:].rearrange("p (g e) -> p g e", e=4)
                    fw.op(V_, lambda g: g.tensor_reduce(out=s["m1"][:], in_=e3, axis=AX.X, op=ALU.max), reads=[s["ee"]], writes=[s["m1"]])
                    fw.op(V_, lambda g: g.tensor_tensor(out=t3, in0=e3, in1=s["m1"][:].unsqueeze(2).to_broadcast([128, 4, 4]), op=ALU.is_lt), reads=[s["ee"], s["m1"]], writes=[s["tt"]])
                    fw.op(V_, lambda g: g.tensor_tensor(out=s["tt"][:], in0=s["tt"][:], in1=s["ee"][:], op=ALU.mult), reads=[s["tt"], s["ee"]], writes=[s["tt"]])
                    fw.op(V_, lambda g: g.tensor_reduce(out=s["m2"][:], in_=t3, axis=AX.X, op=ALU.max), reads=[s["tt"]], writes=[s["m2"]])
                    fw.op(V_, lambda g: g.tensor_tensor(out=s["sc"][:], in0=s["m1"][:], in1=s["m2"][:], op=ALU.add), reads=[s["m1"], s["m2"]], writes=[s["sc"]])
                    fw.op(V_, lambda g: g.reduce_max(out=s["c1"][:, 2:3], in_=s["sc"][:], axis=AX.X), reads=[s["sc"]], writes=[s["c1"]])
                    fw.op(V_, lambda g: g.tensor_scalar(out=s["gm"][:], in0=s["sc"][:], scalar1=s["c1"][:, 2:3], scalar2=None, op0=ALU.is_ge), reads=[s["sc"], s["c1"]], writes=[s["gm"]])
                    fw.op(V_, lambda g: g.tensor_tensor(out=t3, in0=e3, in1=s["m2"][:].unsqueeze(2).to_broadcast([128, 4, 4]), op=ALU.is_ge), reads=[s["ee"], s["m2"]], writes=[s["tt"]])
                    fw.op(V_, lambda g: g.tensor_tensor(out=t3, in0=t3, in1=s["gm"][:].unsqueeze(2).to_broadcast([128, 4, 4]), op=ALU.mult), reads=[s["tt"], s["gm"]], writes=[s["tt"]])
                    fw.op(V_, lambda g: g.reciprocal(out=s["c1"][:, 3:4], in_=s["c1"][:, 2:3]), reads=[s["c1"]], writes=[s["c1"]])
                    fw.op(V_, lambda g: g.scalar_tensor_tensor(out=s["gt"][:], in0=s["ee"][:], scalar=s["c1"][:, 3:4], in1=s["tt"][:], op0=ALU.mult, op1=ALU.mult), reads=[s["ee"], s["c1"], s["tt"]], writes=[s["gt"]])
                    if RM < 3:
                        continue
                    fw.op("pe", lambda g: g.transpose(bk[0:16, 128:256], s["gt"][:], ident[:]), reads=[s["gt"], ident], writes=[bk])
                    fw.op("act", lambda g: g.copy(out=gateT[0:16, i * 128:(i + 1) * 128], in_=bk[0:16, 128:256]), reads=[bk], writes=[gateT])
            ph.close()

        def resid_ln(gB, ph_bufs, l, i, ybanks, lng, lnb, dstA, last_lat_out=False):
            xt, yt, junk, st = ph_bufs
            r = 0 if i < 16 else 1
            for q in range(4):
                sl = slice(q * 512, (q + 1) * 512)
                fw.op("dve", lambda g: g.tensor_tensor(out=yt[:, sl], in0=ybanks[q][:], in1=gB[r][:, sl], op=ALU.mult), reads=[ybanks[q], gB[r]], writes=[yt])
            if debug:
                store_rows("sp", yt, XP, l, i)
            fw.op("dve", lambda g: g.scalar_tensor_tensor(out=xt[:], in0=xt[:], scalar=ALPHA, in1=yt[:], op0=ALU.mult, op1=ALU.add), reads=[xt, yt], writes=[xt])
            layer_norm(xt, junk, st, lng, lnb, yt)
            dst = out if (last_lat_out and i < 16) else dstA
            store_rows("sp", yt, dst, l, i)

        def layer_norm(xt, junk, st, lng, lnb, outb, width=D, eps=LN_EPS, out_ap=None, in_ap=None):
            xin = xt[:] if in_ap is None else in_ap
            fw.op("dve", lambda g: g.reduce_sum(out=st[:, 0:1], in_=xin, axis=AX.X), reads=[xt], writes=[st])
            fw.op("dve", lambda g: g.tensor_scalar(out=st[:, 1:2], in0=st[:, 0:1], scalar1=-1.0 / width, scalar2=None, op0=ALU.mult), reads=[st], writes=[st])
            fw.op("act", lambda g: g.activation(out=junk[:, 0:width], in_=xin, func=AF.Square, bias=st[:, 1:2], scale=1.0, accum_out=st[:, 2:3]), reads=[xt, st], writes=[junk, st])
            fw.op("dve", lambda g: g.tensor_scalar(out=st[:, 3:4], in0=st[:, 2:3], scalar1=1.0 / width, scalar2=eps, op0=ALU.mult, op1=ALU.add), reads=[st], writes=[st])
            fw.op("act", lambda g: g.activation(out=st[:, 4:5], in_=st[:, 3:4], func=AF.Sqrt), reads=[st], writes=[st])
            fw.op("dve", lambda g: g.reciprocal(out=st[:, 5:6], in_=st[:, 4:5]), reads=[st], writes=[st])
            fw.op("dve", lambda g: g.tensor_scalar(out=xin, in0=xin, scalar1=st[:, 1:2], scalar2=st[:, 5:6], op0=ALU.add, op1=ALU.mult), reads=[xt, st], writes=[xt])
            oap = outb[:] if out_ap is None else out_ap
            fw.op("pool", lambda g: g.tensor_tensor(out=xin, in0=xin, in1=lng[:, 0:width], op=ALU.mult), reads=[xt, lng], writes=[xt])
            fw.op("pool", lambda g: g.tensor_tensor(out=oap, in0=xin, in1=lnb[:, 0:width], op=ALU.add), reads=[xt, lnb], writes=[outb])

        def phase_gmlp(l, idx):
            last = l == DEPTH - 1
            ph = Phase(fw)
            hT = ph.sb("hT", [128, KC, T], BF16)
            hres = [Res("hT%d" % i) for i in range(NT)]
            phase_xt(l, hT, hres, 0, 16)
            wb = [ph.sb("wb%d" % i, [128, KC, 512], BF16) for i in range(2)]
            osb = [ph.sb("osb%d" % i, [128, 512], BF16) for i in range(3)]
            vsb = [ph.sb("vsb%d" % i, [128, 512], F32) for i in range(3)]
            wv = gm_w_in[idx].rearrange("(k p) n -> p k n", p=128)
            nb = 0
            no = 0
            for blk in range(8):
                w = wb[blk % 2]
                fw.dma("pool", (w[:], wv[:, :, blk * 512:(blk + 1) * 512]), writes=[w], key=w.ds)
                if blk < 4:
                    for (t0, n) in GROUPS5:
                        for c4 in range(4):
                            bank = pb[nb % 6]
                            nb += 1
                            for k in range(KC):
                                fw.op("pe", lambda g: g.matmul(bank[:, 0:n], lhsT=w[:, k, c4 * 128:(c4 + 1) * 128], rhs=hT[:, k, t0:t0 + n], start=(k == 0), stop=(k == KC - 1)), reads=[w] + hres[t0 // 128:(t0 + n) // 128], writes=[bank])
                            o = osb[no % 3]
                            no += 1
                            fw.op("act", lambda g: g.activation(out=o[:, 0:n], in_=bank[:, 0:n], func=AF.Gelu_apprx_tanh), reads=[bank], writes=[o])
                            ch = blk * 4 + c4
                            nt_ = n // 128
                            fw.dma("sp", (UTs[t0 // 128:t0 // 128 + nt_, :, ch * 128:(ch + 1) * 128].rearrange("t p n -> p t n"), o[:, 0:n].rearrange("p (t n) -> p t n", n=128)), reads=[o], key=o.ds)
                else:
                    for i in range(NT):
                        bank = pb[nb % 6]
                        nb += 1
                        for k in range(KC):
                            fw.op("pe", lambda g: g.matmul(bank[:], lhsT=hT[:, k, i * 128:(i + 1) * 128], rhs=w[:, k, :], start=(k == 0), stop=(k == KC - 1)), reads=[w, hres[i]], writes=[bank])
                        o = vsb[no % 3]
                        no += 1
                        fw.op("act", lambda g: g.activation(out=o[:], in_=bank[:], func=AF.Gelu_apprx_tanh), reads=[bank], writes=[o])
                        fw.dma("sp", (V[i * 128:(i + 1) * 128, (blk - 4) * 512:(blk - 3) * 512], o[:]), reads=[o], key=o.ds)
            ph.close()

            ph = Phase(fw)
            gB = [ph.sb("gB%d" % i, [128, D], F32) for i in range(2)]
            build_gb(gB, 32)
            wo = ph.sb("wo", [128, KC, D], BF16)
            wov = gm_w_out[idx].rearrange("(k p) n -> p k n", p=128)
            woh = [Res("wo%d" % q) for q in range(4)]
            for q in range(4):
                fw.dma("pool", (wo[:, :, q * 512:(q + 1) * 512], wov[:, :, q * 512:(q + 1) * 512]), writes=[woh[q]], key=fw.get_dsem())
            lgB = ph.sb("lgB", [128, D], F32)
            lbB = ph.sb("lbB", [128, D], F32)
            l1g = ph.sb("l1g", [128, D], F32)
            l1b = ph.sb("l1b", [128, D], F32)
            bsB = ph.sb("bsB", [128, KC, 128], F32)
            wst = ph.sb("wst", [128, 8, 128], F32)
            wsT = ph.sb("wsT", [128, 8, 128], BF16)
            fw.dma("sp", (lgB[:], gm_ln_g[idx].partition_broadcast(128)), writes=[lgB], key=lgB.ds)
            fw.dma("sp", (lbB[:], gm_ln_b[idx].partition_broadcast(128)), writes=[lbB], key=lbB.ds)
            fw.dma("sp", (l1g[:], ln1_g[l].partition_broadcast(128)), writes=[l1g], key=l1g.ds)
            fw.dma("sp", (l1b[:], ln1_b[l].partition_broadcast(128)), writes=[l1b], key=l1b.ds)
            bsv = gm_bs[idx].rearrange("g p -> (g p)").partition_broadcast(128).rearrange("q (g p) -> q g p", p=128)
            for dup in range(2):
                fw.dma("sp", (bsB[:].rearrange("q (g d) p -> q g d p", d=2)[:, :, dup, :], bsv), writes=[bsB], key=bsB.ds)
            fw.dma("sp", (wst[:], gm_ws[idx].rearrange("g p q -> p g q")), writes=[wst], key=wst.ds)
            for g_ in range(8):
                bank = pb[g_ % 2]
                fw.op("pe", lambda g: g.transpose(bank[:, 0:128], wst[:, g_, :], ident[:]), reads=[wst, ident], writes=[bank])
                fw.op("dve", lambda g: g.tensor_copy(out=wsT[:, g_, :], in_=bank[:, 0:128]), reads=[bank], writes=[wsT])
            vt = [ph.sb("vt%d" % i, [128, D], F32) for i in range(2)]
            ut = [ph.sb("ut%d" % i, [128, KC, 128], BF16) for i in range(2)]
            xt = [ph.sb("xt%d" % i, [128, D], F32) for i in range(2)]
            yt = [ph.sb("yt%d" % i, [128, D], F32) for i in range(1)] * 2
            vn = [ph.sb("vn%d" % i, [128, D], BF16) for i in range(2)]
            zT = [ph.sb("zT%d" % i, [128, KC, 128], BF16) for i in range(2)]
            stt = [ph.sb("st%d" % i, [128, 8], F32) for i in range(2)]
            st2 = [ph.sb("stb%d" % i, [128, 8], F32) for i in range(2)]
            tmp = [ph.sb("tmp%d" % i, [128, 512], F32) for i in range(2)]
            jkv = ph.sb("jkv", [128, D], BF16)
            ntile = 16 if last else NT

            jk2 = ph.sb("jk2", [128, D], BF16)

            def loadsV(i):
                fw.dma("sp", (vt[i % 2][:], V[i * 128:(i + 1) * 128, :]), writes=[vt[i % 2]], key=vt[i % 2].ds)
                fw.dma("sp", (ut[i % 2][:].rearrange("p k n -> p (k n)"), UTs[i]), writes=[ut[i % 2]], key=ut[i % 2].ds)

            def loadsX(i):
                load_rows("sp", xt[i % 2], X, l, i)

            def stageA(i):
                v_, u_, n_, z_ = vt[i % 2], ut[i % 2], vn[i % 2], zT[i % 2]
                layer_norm(v_, jkv, stt[i % 2], lgB, lbB, n_)
                for q in range(4):
                    bank = pb[q % 2]
                    for cc in range(4):
                        c = q * 4 + cc
                        fw.op("pe", lambda g: g.matmul(bank[:, cc * 128:(cc + 1) * 128], lhsT=n_[:, c * 128:(c + 1) * 128], rhs=wsT[:, c // 2, :], start=True, stop=True), reads=[n_, wsT], writes=[bank])
                    tm = tmp[q % 2]
                    fw.op("dve", lambda g: g.tensor_tensor(out=tm[:], in0=bank[:], in1=bsB[:, q * 4:(q + 1) * 4, :].rearrange("p c n -> p (c n)"), op=ALU.add), reads=[bank, bsB], writes=[tm])
                    fw.op("pool", lambda g: g.tensor_tensor(out=z_[:, q * 4:(q + 1) * 4, :].rearrange("p c n -> p (c n)"), in0=tm[:], in1=u_[:, q * 4:(q + 1) * 4, :].rearrange("p c n -> p (c n)"), op=ALU.mult), reads=[tm, u_], writes=[z_])

            nbs = [0]

            def stageB(i):
                x_, y_, z_ = xt[i % 2], yt[i % 2], zT[i % 2]
                yb = []
                for q in range(4):
                    bank = pb[2 + (nbs[0] % 5)]
                    nbs[0] += 1
                    for k in range(KC):
                        fw.op("pe", lambda g: g.matmul(bank[:], lhsT=z_[:, k, :], rhs=wo[:, k, q * 512:(q + 1) * 512], start=(k == 0), stop=(k == KC - 1)), reads=[z_, woh[q]], writes=[bank])
                    yb.append(bank)
                resid_ln(gB, (x_, y_, jk2, st2[i % 2]), l, i, yb, l1g, l1b, X)

            for j in range(min(2, ntile)):
                loadsV(j)
                loadsX(j)
            stageA(0)
            for i in range(ntile):
                if i + 1 < ntile:
                    stageA(i + 1)
                if i + 2 < ntile:
                    loadsV(i + 2)
                stageB(i)
                if i + 2 < ntile:
                    loadsX(i + 2)
            ph.close()

        def phase_moe(l):
            last = l == DEPTH - 1
            ntile = 16 if last else NT
            groups = GROUPS5[:4] if last else GROUPS5
            ph = Phase(fw)
            sel16 = ph.sb("sel16", [16, 16, 128], F32)
            import os as _os
            XB = int(_os.environ.get("XB", "0"))
            if not (XB & 1):
                fw.op("pool", lambda g: g.memset(sel16[:], 0.0), writes=[sel16])
                fw.op("pool", lambda g: g.affine_select(out=sel16[:], in_=sel16[:], pattern=[[-1, 16], [0, 128]], compare_op=ALU.not_equal, fill=1.0, base=0, channel_multiplier=1), reads=[sel16], writes=[sel16])
            hT = ph.sb("hT", [128, KC, T], BF16)
            hres = [Res("hT%d" % i) for i in range(NT)]
            gateT = ph.sb("gateT", [16, T], F32)
            phase_xt(l, hT, hres, 48, 64, router=gateT)
            if stop == ("moe_xt", l):
                ph.close()
                return
            wg = [ph.sb("wg%d" % i, [128, KC, 512], BF16) for i in range(2)]
            wu = [ph.sb("wu%d" % i, [128, KC, 512], BF16) for i in range(2)]
            gbs = [ph.sb("gbs%d" % i, [128, 512], F32) for i in range(2)]
            sa = [ph.sb("sa%d" % i, [128, 512], F32) for i in range(2)]
            hb = [ph.sb("hb%d" % i, [128, 512], BF16) for i in range(3)]
            nh = 0
            ng = 0
            for e in range(NEXP):
                g_, u_ = wg[e % 2], wu[e % 2]
                fw.dma("pool", (g_[:], ex_w_gate[l, e].rearrange("(k p) n -> p k n", p=128)), writes=[g_], key=g_.ds)
                fw.dma("pool", (u_[:], ex_w_up[l, e].rearrange("(k p) n -> p k n", p=128)), writes=[u_], key=u_.ds)
                for (t0, n) in groups:
                    gbk = pb[6]
                    gb_ = gbs[ng % 2]
                    ng += 1
                    fw.op("pe", lambda g: g.matmul(gbk[:, 0:n], lhsT=sel16[:, e, :], rhs=gateT[0:16, t0:t0 + n], start=True, stop=True), reads=[sel16, gateT], writes=[gbk])
                    fw.op("act", lambda g: g.copy(out=gb_[:, 0:n], in_=gbk[:, 0:n]), reads=[gbk], writes=[gb_])
                    for fc in range(4):
                        ba = pb[(nh % 3) * 2]
                        bu = pb[(nh % 3) * 2 + 1]
                        for k in range(KC):
                            fw.op("pe", lambda g: g.matmul(ba[:, 0:n], lhsT=g_[:, k, fc * 128:(fc + 1) * 128], rhs=hT[:, k, t0:t0 + n], start=(k == 0), stop=(k == KC - 1)), reads=[g_] + hres[t0 // 128:(t0 + n) // 128], writes=[ba])
                        for k in range(KC):
                            fw.op("pe", lambda g: g.matmul(bu[:, 0:n], lhsT=u_[:, k, fc * 128:(fc + 1) * 128], rhs=hT[:, k, t0:t0 + n], start=(k == 0), stop=(k == KC - 1)), reads=[u_] + hres[t0 // 128:(t0 + n) // 128], writes=[bu])
                        s_ = sa[nh % 2]
                        h_ = hb[nh % 3]
                        nh += 1
                        fw.op("act", lambda g: g.activation(out=s_[:, 0:n], in_=ba[:, 0:n], func=AF.Silu), reads=[ba], writes=[s_])
                        fw.op("pool", lambda g: g.tensor_tensor(out=s_[:, 0:n], in0=s_[:, 0:n], in1=gb_[:, 0:n], op=ALU.mult), reads=[s_, gb_], writes=[s_])
                        fw.op("dve", lambda g: g.tensor_tensor(out=h_[:, 0:n], in0=bu[:, 0:n], in1=s_[:, 0:n], op=ALU.mult), reads=[bu, s_], writes=[h_])
                        ch = e * 4 + fc
                        nt_ = n // 128
                        fw.dma("sp", (HTs[t0 // 128:t0 // 128 + nt_, :, ch * 128:(ch + 1) * 128].rearrange("t p n -> p t n"), h_[:, 0:n].rearrange("p (t n) -> p t n", n=128)), reads=[h_], key=h_.ds)
            ph.close()

            if stop == ("moe_m2", l):
                return
            ph = Phase(fw)
            gB = [ph.sb("gB%d" % i, [128, D], F32) for i in range(2)]
            build_gb(gB, 80)
            wd = [ph.sb("wd%d" % i, [128, 32, 512], BF16) for i in range(2)]
            ht = [ph.sb("ht%d" % i, [128, 64, 128], BF16) for i in range(3)]
            xq = [ph.sb("xq%d" % i, [128, 512], F32) for i in range(3)]
            yq = [ph.sb("yq%d" % i, [128, 512], F32) for i in range(3)]
            wdv = ex_w_down[l].rearrange("e (fc p) n -> p (e fc) n", p=128)
            n = 0
            seq = [(dt_, i) for dt_ in range(4) for i in range(ntile)]

            def m3_loads(j):
                dt2, i2 = seq[j]
                fw.dma("sp", (ht[j % 3][:].rearrange("p k n -> p (k n)"), HTs[i2]), writes=[ht[j % 3]], key=ht[j % 3].ds)
                load_rows("sp", xq[j % 3], X, l, i2, dt2 * 512, (dt2 + 1) * 512)
            m3_loads(0)
            for dt_ in range(4):
                sl = slice(dt_ * 512, (dt_ + 1) * 512)
                for hf in range(2):
                    fw.dma("pool", (wd[hf][:], wdv[:, hf * 32:(hf + 1) * 32, sl]), writes=[wd[hf]], key=wd[hf].ds)
                for i in range(ntile):
                    r = 0 if i < 16 else 1
                    h_ = ht[n % 3]
                    x_ = xq[n % 3]
                    y_ = yq[n % 3]
                    bank = pb[n % 6]
                    n += 1
                    if n < len(seq):
                        m3_loads(n)
                    for j in range(64):
                        fw.op("pe", lambda g: g.matmul(bank[:], lhsT=h_[:, j, :], rhs=wd[j // 32][:, j % 32, :], start=(j == 0), stop=(j == 63)), reads=[h_, wd[j // 32]], writes=[bank])
                    fw.op("dve", lambda g: g.tensor_tensor(out=y_[:], in0=bank[:], in1=gB[r][:, sl], op=ALU.mult), reads=[bank, gB[r]], writes=[y_])
                    fw.op("dve", lambda g: g.scalar_tensor_tensor(out=y_[:], in0=x_[:], scalar=ALPHA, in1=y_[:], op0=ALU.mult, op1=ALU.add), reads=[x_, y_], writes=[y_])
                    store_rows("sp", y_, XP, l, i, dt_ * 512, (dt_ + 1) * 512)
            ph.close()

            if stop == ("moe_m3", l):
                return
            ph = Phase(fw)
            gens = [gen_ln2(l, ph, ntile, last)]
            if l + 1 < nlayers:
                gens.append(gen_ada(l + 1, ph))
            while gens:
                for g_ in list(gens):
                    try:
                        next(g_)
                    except StopIteration:
                        gens.remove(g_)
            ph.close()

        def gen_ln2(l, ph, ntile, last):
            l2g = ph.sb("l2g", [128, D], F32)
            l2b = ph.sb("l2b", [128, D], F32)
            fw.dma("sp", (l2g[:], ln2_g[l].partition_broadcast(128)), writes=[l2g], key=l2g.ds)
            fw.dma("sp", (l2b[:], ln2_b[l].partition_broadcast(128)), writes=[l2b], key=l2b.ds)
            xt = [ph.sb("xt%d" % i, [128, D], F32) for i in range(3)]
            yt = [ph.sb("yt%d" % i, [128, D], F32) for i in range(2)]
            jk = ph.sb("jk", [128, D], BF16)
            stt = [ph.sb("st%d" % i, [128, 8], F32) for i in range(2)]
            load_rows("sp", xt[0], XP, l, 0)
            for i in range(ntile):
                if i + 1 < ntile:
                    load_rows("sp", xt[(i + 1) % 3], XP, l, i + 1)
                layer_norm(xt[i % 3], jk, stt[i % 2], l2g, l2b, yt[i % 2])
                store_rows("sp", yt[i % 2], out if (last and i < 16) else X, l, i)
                yield

        def phase_mlstm(l, idx):
            last = l == DEPTH - 1
            W = ml_w_in[idx].rearrange("(k p) n -> p k n", p=128)
            P2 = Phase(fw)
            NCH = NT
            sel4 = P2.sb("sel4", [4, 4, 128], F32)
            COL = P2.sb("COL", [128, NCH, 32], F32)
            ACOL = P2.sb("ACOL", [128, 8, NCH], F32)
            BETA = [P2.sb("BETA%d" % d_, [4, T], F32) for d_ in range(2)]
            PR = Phase(fw)
            rows = {}
            for nm in ("I", "F"):
                for d_ in range(2):
                    rows[(nm, d_)] = PR.sb("row%s%d" % (nm, d_), [4, T], F32)
            ph = Phase(fw)
            hT = ph.sb("hT", [128, KC, T], BF16)
            hres = [Res("hT%d" % i) for i in range(NT)]
            phase_xt(l, hT, hres, 0, 16)
            wgt = ph.sb("wgt", [128, KC, 16], BF16)
            fw.dma("pool", (wgt[:], W[:, :, 6144:6160]), writes=[wgt], key=wgt.ds)
            gbt = ph.sb("gbt", [4, 4], F32)
            fw.dma("sp", (gbt[:], ml_gate_b[idx].rearrange("(a h) -> h a", h=4)), writes=[gbt], key=gbt.ds, allow_slow_non_contiguous=True)
            for (nm, d_, c0, a_) in (("I", 0, 0, 0), ("F", 0, 4, 1), ("I", 1, 8, 2), ("F", 1, 12, 3)):
                rw_ = rows[(nm, d_)]
                for gi, (t0, n) in enumerate(GROUPS5):
                    bank = pb[gi % 2]
                    for k in range(KC):
                        fw.op("pe", lambda g: g.matmul(bank[0:4, 0:n], lhsT=wgt[:, k, c0:c0 + 4], rhs=hT[:, k, t0:t0 + n], start=(k == 0), stop=(k == KC - 1)), reads=[wgt] + hres[t0 // 128:(t0 + n) // 128], writes=[bank])
                    fw.op("act", lambda g: g.activation(out=rw_[:, t0:t0 + n], in_=bank[0:4, 0:n], func=AF.Identity, bias=gbt[:, a_:a_ + 1], scale=1.0), reads=[bank, gbt], writes=[rw_])
            cvt = ph.sb("cvt", [48, 128], F32)
            cvT = ph.sb("cvT", [128, 48], F32)
            fw.dma("sp", (cvt[:], ml_conv[idx].rearrange("j (c p) -> (j c) p", p=128)), writes=[cvt], key=cvt.ds)
            fw.op("pe", lambda g: g.transpose(pb[2][:, 0:48], cvt[:], ident[0:48, 0:48]), reads=[cvt, ident], writes=[pb[2]])
            fw.op("dve", lambda g: g.tensor_copy(out=cvT[:], in_=pb[2][:, 0:48]), reads=[pb[2]], writes=[cvT])
            wb = [ph.sb("wb%d" % i, [128, KC, 512], BF16) for i in range(2)]
            praw = [ph.sb("praw%d" % i, [128, T + 4], F32) for i in range(1)] * 2
            acc = [ph.sb("acc%d" % i, [128, T], F32) for i in range(1)] * 2
            qo = [ph.sb("qo%d" % i, [128, T], BF16) for i in range(2)]
            for p_ in praw:
                fw.op("pool", lambda g: g.memset(p_[:], 0.0), writes=[p_])
            nb = 0
            nq = 0
            for blk in range(4):
                w = wb[blk % 2]
                fw.dma("pool", (w[:], W[:, :, blk * 512:(blk + 1) * 512]), writes=[w], key=w.ds)
                for c4 in range(4):
                    ch = blk * 4 + c4
                    p_ = praw[nq % 2]
                    a_ = acc[nq % 2]
                    o_ = qo[nq % 2]
                    nq += 1
                    for (t0, n) in GROUPS5:
                        bank = pb[nb % 6]
                        nb += 1
                        for k in range(KC):
                            fw.op("pe", lambda g: g.matmul(bank[:, 0:n], lhsT=w[:, k, c4 * 128:(c4 + 1) * 128], rhs=hT[:, k, t0:t0 + n], start=(k == 0), stop=(k == KC - 1)), reads=[w] + hres[t0 // 128:(t0 + n) // 128], writes=[bank])
                        off = 1 + t0 + (2 if t0 >= TL else 0)
                        fw.op("act", lambda g: g.copy(out=p_[:, off:off + n], in_=bank[:, 0:n]), reads=[bank], writes=[p_], skip_self=True)
                    for (s0, n, off) in ((0, TL, 1), (TL, TCX, TL + 3)):
                        fw.op("dve", lambda g: g.tensor_scalar(out=a_[:, s0:s0 + n], in0=p_[:, off:off + n], scalar1=cvT[:, 16 + ch:17 + ch], scalar2=None, op0=ALU.mult), reads=[p_, cvT], writes=[a_])
                        fw.op("dve", lambda g: g.scalar_tensor_tensor(out=a_[:, s0:s0 + n], in0=p_[:, off - 1:off - 1 + n], scalar=cvT[:, ch:ch + 1], in1=a_[:, s0:s0 + n], op0=ALU.mult, op1=ALU.add), reads=[p_, cvT, a_], writes=[a_])
                        fw.op("dve", lambda g: g.scalar_tensor_tensor(out=a_[:, s0:s0 + n], in0=p_[:, off + 1:off + 1 + n], scalar=cvT[:, 32 + ch:33 + ch], in1=a_[:, s0:s0 + n], op0=ALU.mult, op1=ALU.add), reads=[p_, cvT, a_], writes=[a_])
                    if ch < 8:
                        fw.op("act", lambda g: g.activation(out=o_[:], in_=a_[:], func=AF.Silu), reads=[a_], writes=[o_])
                    else:
                        fw.op("act", lambda g: g.activation(out=a_[:], in_=a_[:], func=AF.Silu), reads=[a_], writes=[a_])
                        fw.op("pool", lambda g: g.tensor_scalar(out=o_[:], in0=a_[:], scalar1=0.0625, scalar2=None, op0=ALU.mult), reads=[a_], writes=[o_])
                    fw.dma("sp", (QK[ch * 128:(ch + 1) * 128, :], o_[:]), reads=[o_], key=o_.ds)
            osb = [ph.sb("osb%d" % i, [128, 512], BF16) for i in range(3)]
            vsb = [ph.sb("vsb%d" % i, [128, 512], F32) for i in range(3)]
            no = 0
            for blk in range(4, 12):
                w = wb[blk % 2]
                fw.dma("pool", (w[:], W[:, :, blk * 512:(blk + 1) * 512]), writes=[w], key=w.ds)
                isv = blk < 8
                for i in range(NT if (isv or not last) else 16):
                    bank = pb[nb % 6]
                    nb += 1
                    for k in range(KC):
                        fw.op("pe", lambda g: g.matmul(bank[:], lhsT=hT[:, k, i * 128:(i + 1) * 128], rhs=w[:, k, :], start=(k == 0), stop=(k == KC - 1)), reads=[w, hres[i]], writes=[bank])
                    if isv:
                        o = osb[no % 3]
                        fw.op("act", lambda g: g.copy(out=o[:], in_=bank[:]), reads=[bank], writes=[o])
                        fw.dma("sp", (VS[i * 128:(i + 1) * 128, (blk - 4) * 512:(blk - 3) * 512], o[:]), reads=[o], key=o.ds)
                    else:
                        o = vsb[no % 3]
                        fw.op("act", lambda g: g.activation(out=o[:], in_=bank[:], func=AF.Sigmoid), reads=[bank], writes=[o])
                        fw.dma("sp", (V[i * 128:(i + 1) * 128, (blk - 8) * 512:(blk - 7) * 512], o[:]), reads=[o], key=o.ds)
                    no += 1
            ph.close()

            ph = Phase(fw)
            fw.op("pool", lambda g: g.memset(sel4[:], 0.0), writes=[sel4])
            fw.op("pool", lambda g: g.affine_select(out=sel4[:], in_=sel4[:], pattern=[[-1, 4], [0, 128]], compare_op=ALU.not_equal, fill=1.0, base=0, channel_multiplier=1), reads=[sel4], writes=[sel4])
            rmask = ph.sb("rmask", [4, T], F32)
            rneg = ph.sb("rneg", [4, T], F32)
            fw.op("pool", lambda g: g.memset(rmask[:], 1.0), writes=[rmask])
            fw.op("pool", lambda g: g.memset(rmask[:].rearrange("p (c t) -> p c t", t=128)[:, :, 0:1], 0.0), reads=[rmask], writes=[rmask])
            fw.op("pool", lambda g: g.memset(rneg[:], 0.0), writes=[rneg])
            fw.op("pool", lambda g: g.memset(rneg[:].rearrange("p (c t) -> p c t", t=128)[:, :, 0:1], -1e30), reads=[rneg], writes=[rneg])
            i4 = ident
            for d_ in range(2):
                I_, F_ = rows[("I", d_)], rows[("F", d_)]
                ph2 = ph
                ph = Phase(fw)
                B_ = ph.sb("B_%d" % d_, [4, T], F32)
                U_ = ph.sb("U_%d" % d_, [4, T], F32)
                CM = ph.sb("CM%d" % d_, [4, T], F32)
                TM = ph.sb("TM%d" % d_, [4, T], F32)
                DEC = ph.sb("DEC%d" % d_, [4, T], F32)
                ENG = ph.sb("ENG%d" % d_, [4, T], F32)
                KWS = ph.sb("KWS%d" % d_, [4, T], F32)
                BL = ph.sb("BL%d" % d_, [4, NCH], F32)
                UM = ph.sb("UM%d" % d_, [4, NCH], F32)
                MN = ph.sb("MN%d" % d_, [4, NCH], F32)
                MP = ph.sb("MP%d" % d_, [4, NCH], F32)
                AA = ph.sb("AA%d" % d_, [4, NCH], F32)
                rv = (lambda ap: ap) if d_ == 0 else (lambda ap: ap[:, ::-1])
                fw.op("act", lambda g: g.activation(out=F_[:], in_=F_[:], func=AF.Exp, scale=-1.0), reads=[F_], writes=[F_])
                fw.op("act", lambda g: g.activation(out=F_[:], in_=F_[:], func=AF.Ln, bias=1.0, scale=1.0), reads=[F_], writes=[F_])
                fw.op("dve", lambda g: g.tensor_scalar(out=F_[:], in0=F_[:], scalar1=-1.0, scalar2=None, op0=ALU.mult), reads=[F_], writes=[F_])
                for (s0, n) in ((0, TL), (TL, TCX)):
                    fw.op("dve", lambda g: g.tensor_tensor_scan(out=rv(B_[:, s0:s0 + n]), data0=rmask[:, 0:n], data1=rv(F_[:, s0:s0 + n]), initial=0.0, op0=ALU.mult, op1=ALU.add), reads=[F_, rmask], writes=[B_])
                fw.op("dve", lambda g: g.tensor_tensor(out=U_[:], in0=I_[:], in1=B_[:], op=ALU.subtract), reads=[I_, B_], writes=[U_])
                for (s0, n) in ((0, TL), (TL, TCX)):
                    fw.op("dve", lambda g: g.tensor_tensor_scan(out=rv(CM[:, s0:s0 + n]), data0=rneg[:, 0:n], data1=rv(U_[:, s0:s0 + n]), initial=-1e30, op0=ALU.add, op1=ALU.max), reads=[U_, rneg], writes=[CM])
                lastpos = 127 if d_ == 0 else 0
                b3 = B_[:].rearrange("p (c t) -> p c t", t=128)
                c3 = CM[:].rearrange("p (c t) -> p c t", t=128)
                fw.op("dve", lambda g: g.tensor_copy(out=BL[:], in_=b3[:, :, lastpos]), reads=[B_], writes=[BL])
                fw.op("dve", lambda g: g.tensor_copy(out=UM[:], in_=c3[:, :, lastpos]), reads=[CM], writes=[UM])
                if d_ == 0:
                    segs = [(16, 18, False), (0, 16, False)]
                else:
                    segs = [(16, 18, True), (0, 16, True)]
                init = 0.0
                for (a0, a1, rev) in segs:
                    sv = (lambda ap: ap[:, ::-1]) if rev else (lambda ap: ap)
                    ini = init
                    fw.op("dve", lambda g: g.tensor_tensor_scan(out=sv(MN[:, a0:a1]), data0=sv(UM[:, a0:a1]), data1=sv(BL[:, a0:a1]), initial=ini, op0=ALU.max, op1=ALU.add), reads=[UM, BL, MN], writes=[MN])
                    init = MN[:, (a0 if rev else a1 - 1):(a0 if rev else a1 - 1) + 1]
                if d_ == 0:
                    fw.op("dve", lambda g: g.memset(MP[:, 16:17], 0.0), writes=[MP])
                    fw.op("dve", lambda g: g.tensor_copy(out=MP[:, 17:18], in_=MN[:, 16:17]), reads=[MN], writes=[MP])
                    fw.op("dve", lambda g: g.tensor_copy(out=MP[:, 0:1], in_=MN[:, 17:18]), reads=[MN, MP], writes=[MP])
                    fw.op("dve", lambda g: g.tensor_copy(out=MP[:, 1:16], in_=MN[:, 0:15]), reads=[MN, MP], writes=[MP])
                else:
                    fw.op("dve", lambda g: g.memset(MP[:, 17:18], 0.0), writes=[MP])
                    fw.op("dve", lambda g: g.tensor_copy(out=MP[:, 16:17], in_=MN[:, 17:18]), reads=[MN], writes=[MP])
                    fw.op("dve", lambda g: g.tensor_copy(out=MP[:, 15:16], in_=MN[:, 16:17]), reads=[MN, MP], writes=[MP])
                    fw.op("dve", lambda g: g.tensor_copy(out=MP[:, 0:15], in_=MN[:, 1:16]), reads=[MN, MP], writes=[MP])
                mpb = MP[:].unsqueeze(2).to_broadcast([4, NCH, 128])
                t3 = TM[:].rearrange("p (c t) -> p c t", t=128)
                be3 = BETA[d_][:].rearrange("p (c t) -> p c t", t=128)
                fw.op("dve", lambda g: g.tensor_tensor(out=t3, in0=c3, in1=mpb, op=ALU.max), reads=[CM, MP], writes=[TM])
                fw.op("dve", lambda g: g.tensor_scalar(out=BETA[d_][:], in0=TM[:], scalar1=-1.0, scalar2=None, op0=ALU.mult), reads=[TM], writes=[BETA[d_]])
                fw.op("dve", lambda g: g.tensor_tensor(out=t3, in0=be3, in1=mpb, op=ALU.add), reads=[BETA[d_], MP, TM], writes=[TM])
                fw.op("act", lambda g: g.activation(out=DEC[:], in_=TM[:], func=AF.Exp), reads=[TM], writes=[DEC])
                fw.op("dve", lambda g: g.tensor_tensor(out=TM[:], in0=BETA[d_][:], in1=B_[:], op=ALU.subtract), reads=[BETA[d_], B_, DEC], writes=[TM])
                fw.op("act", lambda g: g.activation(out=ENG[:], in_=TM[:], func=AF.Exp), reads=[TM], writes=[ENG])
                fw.op("dve", lambda g: g.tensor_tensor(out=AA[:], in0=BL[:], in1=MN[:], op=ALU.subtract), reads=[BL, MN], writes=[AA])
                fw.op("dve", lambda g: g.tensor_tensor(out=t3, in0=U_[:].rearrange("p (c t) -> p c t", t=128), in1=AA[:].unsqueeze(2).to_broadcast([4, NCH, 128]), op=ALU.add), reads=[U_, AA, ENG], writes=[TM])
                fw.op("act", lambda g: g.activation(out=KWS[:], in_=TM[:], func=AF.Exp), reads=[TM], writes=[KWS])
                fw.op("dve", lambda g: g.tensor_tensor(out=AA[:], in0=AA[:], in1=MP[:], op=ALU.add), reads=[AA, MP], writes=[AA])
                fw.op("act", lambda g: g.activation(out=AA[:], in_=AA[:], func=AF.Exp), reads=[AA], writes=[AA])
                for c in range(NCH):
                    bank = pb[c % 2]
                    for qi, Q_ in enumerate((U_, DEC, ENG, KWS)):
                        fw.op("pe", lambda g: g.matmul(bank[:, qi * 4:qi * 4 + 4], lhsT=Q_[:, c * 128:(c + 1) * 128], rhs=i4[0:4, 0:4], start=True, stop=True), reads=[Q_, ident], writes=[bank])
                    fw.op("act", lambda g: g.copy(out=COL[:, c, :].rearrange("p (q e) -> p q e", e=8)[:, :, d_ * 4:d_ * 4 + 4], in_=bank[:, 0:16].rearrange("p (q e) -> p q e", e=4)), reads=[bank], writes=[COL])
                for h in range(4):
                    bank = pb[2 + h % 2]
                    fw.op("pe", lambda g: g.matmul(bank[:, 0:NCH], lhsT=sel4[:, h, :], rhs=AA[:], start=True, stop=True), reads=[sel4, AA], writes=[bank])
                    fw.op("act", lambda g: g.copy(out=ACOL[:, d_ * 4 + h, :], in_=bank[:, 0:NCH]), reads=[bank], writes=[ACOL])
                ph.close()
                ph = ph2
            ph.close()
            PR.close()

            ph = Phase(fw)
            ngB = ph.sb("ngB", [128, D], F32)
            fw.dma("sp", (ngB[:], ml_norm_g[idx].partition_broadcast(128)), writes=[ngB], key=ngB.ds)
            ntile_out = 16 if last else NT
            for h in range(4):
                hp = Phase(fw)
                qT = hp.sb("qT", [128, 2, T], BF16)
                kT = hp.sb("kT", [128, 2, T], BF16)
                vh = hp.sb("vh", [128, NT, 512], BF16)
                kt = hp.sb("kt", [128, NT, 256], BF16)
                hacc = hp.sb("hacc", [128, NT, 512], F32)
                hres_ = [Res("hacc%d" % i) for i in range(NT)]
                for dc in range(2):
                    fw.dma("sp", (qT[:, dc, :], QK[h * 256 + dc * 128:h * 256 + (dc + 1) * 128, :]), writes=[qT], key=qT.ds)
                    fw.dma("sp", (kT[:, dc, :], QK[1024 + h * 256 + dc * 128:1024 + h * 256 + (dc + 1) * 128, :]), writes=[kT], key=kT.ds)
                fw.dma("sp", (vh[:], VS[:, h * 512:(h + 1) * 512].rearrange("(c p) n -> p c n", p=128)), writes=[vh], key=vh.ds)
                for c in range(NT):
                    for dc in range(2):
                        fw.op("pe", lambda g: g.transpose(pbh[:, (c % 4) * 256 + dc * 128:(c % 4) * 256 + (dc + 1) * 128], kT[:, dc, c * 128:(c + 1) * 128], identb[:]), reads=[kT, identb], writes=[pbh])
                    fw.op("act", lambda g: g.copy(out=kt[:, c, :], in_=pbh[:, (c % 4) * 256:(c % 4 + 1) * 256]), reads=[pbh], writes=[kt])
                st = {}
                for d_ in range(2):
                    s = {}
                    s["C"] = hp.sb("C%d" % d_, [128, 2, 512], F32)
                    s["Cb"] = hp.sb("Cb%d" % d_, [128, 2, 512], BF16)
                    s["n"] = hp.sb("n%d" % d_, [128, 2], F32)
                    s["nb"] = hp.sb("nb%d" % d_, [128, 2], BF16)
                    s["ET"] = hp.sb("ET%d" % d_, [128, 128], F32)
                    s["ST"] = hp.sb("ST%d" % d_, [128, 128], BF16)
                    s["pd"] = hp.sb("pd%d" % d_, [128, 8], F32)
                    s["t1"] = hp.sb("t1%d" % d_, [128, 512], F32)
                    s["tm"] = hp.sb("tm%d" % d_, [128, 512], F32)
                    s["kw"] = hp.sb("kw%d" % d_, [128, 256], BF16)
                    s["bk"] = [pb[d_ * 3], pb[d_ * 3 + 1], pb[d_ * 3 + 2]]
                    for nm in ("C", "Cb", "n", "nb"):
                        b_ = s[nm]
                        fw.op("pool", lambda g: g.memset(b_[:], 0.0), writes=[b_])
                    st[d_] = s
                order = {0: [16, 17] + list(range(16)), 1: [17, 16] + list(range(15, -1, -1))}
                hwritten = set()
                for step in range(NT):
                    for d_ in range(2):
                        s = st[d_]
                        c = order[d_][step]
                        tk = slice(c * 128, (c + 1) * 128)
                        bA, bB, bC = s["bk"]
                        colu = COL[:, c, 0 * 8 + d_ * 4 + h:0 * 8 + d_ * 4 + h + 1]
                        cold = COL[:, c, 1 * 8 + d_ * 4 + h:1 * 8 + d_ * 4 + h + 1]
                        cole = COL[:, c, 2 * 8 + d_ * 4 + h:2 * 8 + d_ * 4 + h + 1]
                        colk = COL[:, c, 3 * 8 + d_ * 4 + h:3 * 8 + d_ * 4 + h + 1]
                        cola = ACOL[:, d_ * 4 + h, c:c + 1]
                        msk = masklo if d_ == 0 else maskhi
                        fw.op("pe", lambda g: g.matmul(bA[:, 0:128], lhsT=sel4[:, h, :], rhs=BETA[d_][:, tk], start=True, stop=True), reads=[sel4, BETA[d_]], writes=[bA])
                        for dc in range(2):
                            fw.op("pe", lambda g: g.matmul(bA[:, 128:256], lhsT=kT[:, dc, tk], rhs=qT[:, dc, tk], start=(dc == 0), stop=(dc == 1)), reads=[kT, qT], writes=[bA])
                        fw.op("act", lambda g: g.activation(out=s["ET"][:], in_=bA[:, 0:128], func=AF.Exp, bias=colu, scale=1.0), reads=[bA, COL], writes=[s["ET"]])
                        fw.op("pool", lambda g: g.tensor_tensor(out=s["ET"][:], in0=s["ET"][:], in1=msk[:], op=ALU.mult), reads=[s["ET"], msk], writes=[s["ET"]])
                        fw.op("dve", lambda g: g.tensor_tensor(out=s["ST"][:], in0=bA[:, 128:256], in1=s["ET"][:], op=ALU.mult), reads=[bA, s["ET"]], writes=[s["ST"]])
                        for dc in range(2):
                            fw.op("pe", lambda g: g.matmul(bA[:, 256:257], lhsT=qT[:, dc, tk], rhs=s["nb"][:, dc:dc + 1], start=(dc == 0), stop=(dc == 1)), reads=[qT, s["nb"]], writes=[bA])
                        fw.op("pe", lambda g: g.matmul(bA[:, 257:258], lhsT=s["ST"][:], rhs=onesb[:, 0:1], start=True, stop=True), reads=[s["ST"], onesb], writes=[bA])
                        for dc in range(2):
                            fw.op("pe", lambda g: g.matmul(bB[:], lhsT=qT[:, dc, tk], rhs=s["Cb"][:, dc, :], start=(dc == 0), stop=(dc == 1)), reads=[qT, s["Cb"]], writes=[bB])
                        fw.op("pe", lambda g: g.matmul(bC[:], lhsT=s["ST"][:], rhs=vh[:, c, :], start=True, stop=True), reads=[s["ST"], vh], writes=[bC])
                        pd = s["pd"]
                        fw.op("act", lambda g: g.copy(out=pd[:, 0:2], in_=bA[:, 256:258]), reads=[bA], writes=[pd])
                        fw.op("dve", lambda g: g.scalar_tensor_tensor(out=pd[:, 2:3], in0=pd[:, 0:1], scalar=cold, in1=pd[:, 1:2], op0=ALU.mult, op1=ALU.add), reads=[pd, COL], writes=[pd])
                        fw.op("dve", lambda g: g.tensor_scalar(out=pd[:, 3:4], in0=pd[:, 2:3], scalar1=-1.0, scalar2=None, op0=ALU.mult), reads=[pd], writes=[pd])
                        fw.op("dve", lambda g: g.tensor_tensor(out=pd[:, 3:4], in0=pd[:, 3:4], in1=pd[:, 2:3], op=ALU.max), reads=[pd], writes=[pd])
                        fw.op("dve", lambda g: g.tensor_tensor(out=pd[:, 4:5], in0=pd[:, 3:4], in1=cole, op=ALU.max), reads=[pd, COL], writes=[pd])
                        fw.op("dve", lambda g: g.reciprocal(out=pd[:, 5:6], in_=pd[:, 4:5]), reads=[pd], writes=[pd])
                        fw.op("dve", lambda g: g.tensor_tensor(out=pd[:, 6:7], in0=pd[:, 5:6], in1=cold, op=ALU.mult), reads=[pd, COL], writes=[pd])
                        fw.op("act", lambda g: g.activation(out=s["t1"][:], in_=bB[:], func=AF.Copy, scale=pd[:, 6:7]), reads=[bB, pd], writes=[s["t1"]])
                        if c not in hwritten:
                            hwritten.add(c)
                            fw.op("dve", lambda g: g.scalar_tensor_tensor(out=hacc[:, c, :], in0=bC[:], scalar=pd[:, 5:6], in1=s["t1"][:], op0=ALU.mult, op1=ALU.add), reads=[bC, pd, s["t1"]], writes=[hres_[c]])
                        else:
                            fw.op("dve", lambda g: g.scalar_tensor_tensor(out=s["tm"][:], in0=bC[:], scalar=pd[:, 5:6], in1=s["t1"][:], op0=ALU.mult, op1=ALU.add), reads=[bC, pd, s["t1"]], writes=[s["tm"]])
                            fw.op("pool", lambda g: g.tensor_tensor(out=hacc[:, c, :], in0=hacc[:, c, :], in1=s["tm"][:], op=ALU.add), reads=[s["tm"], hres_[c]], writes=[hres_[c]])
                        if step < NT - 1:
                            fw.op("pool", lambda g: g.tensor_scalar(out=s["kw"][:], in0=kt[:, c, :], scalar1=colk, scalar2=None, op0=ALU.mult), reads=[kt, COL], writes=[s["kw"]])
                            for dc, bk_ in ((0, bB), (1, bC)):
                                fw.op("pe", lambda g: g.matmul(bk_[:], lhsT=s["kw"][:, dc * 128:(dc + 1) * 128], rhs=vh[:, c, :], start=True, stop=True), reads=[s["kw"], vh], writes=[bk_])
                                fw.op("pe", lambda g: g.matmul(bA[:, 260 + dc:261 + dc], lhsT=s["kw"][:, dc * 128:(dc + 1) * 128], rhs=onesb[:, 0:1], start=True, stop=True), reads=[s["kw"], onesb], writes=[bA])
                            for dc, bk_ in ((0, bB), (1, bC)):
                                fw.op("dve", lambda g: g.scalar_tensor_tensor(out=s["C"][:, dc, :], in0=s["C"][:, dc, :], scalar=cola, in1=bk_[:], op0=ALU.mult, op1=ALU.add), reads=[s["C"], ACOL, bk_], writes=[s["C"]])
                            fw.op("act", lambda g: g.copy(out=s["Cb"][:], in_=s["C"][:]), reads=[s["C"]], writes=[s["Cb"]])
                            fw.op("dve", lambda g: g.scalar_tensor_tensor(out=s["n"][:], in0=s["n"][:], scalar=cola, in1=bA[:, 260:262], op0=ALU.mult, op1=ALU.add), reads=[s["n"], ACOL, bA], writes=[s["n"]])
                            fw.op("act", lambda g: g.copy(out=s["nb"][:], in_=s["n"][:]), reads=[s["n"]], writes=[s["nb"]])
                og = [hp.sb("og%d" % i, [128, 512], F32) for i in range(2)]
                zo = [hp.sb("zo%d" % i, [128, 512], BF16) for i in range(2)]
                jk = hp.sb("jk", [128, 512], BF16)
                ss = [hp.sb("ss%d" % i, [128, 8], F32) for i in range(2)]
                for i in range(ntile_out):
                    o_, z_, s_ = og[i % 2], zo[i % 2], ss[i % 2]
                    fw.dma("sp", (o_[:], V[i * 128:(i + 1) * 128, h * 512:(h + 1) * 512]), writes=[o_], key=o_.ds)
                    hv = hacc[:, i, :]
                    fw.op("act", lambda g: g.activation(out=jk[:], in_=hv, func=AF.Square, accum_out=s_[:, 0:1]), reads=[hres_[i]], writes=[jk, s_])
                    fw.op("dve", lambda g: g.tensor_scalar(out=s_[:, 1:2], in0=s_[:, 0:1], scalar1=1.0 / 512, scalar2=RMS_EPS, op0=ALU.mult, op1=ALU.add), reads=[s_], writes=[s_])
                    fw.op("act", lambda g: g.activation(out=s_[:, 2:3], in_=s_[:, 1:2], func=AF.Sqrt), reads=[s_], writes=[s_])
                    fw.op("dve", lambda g: g.reciprocal(out=s_[:, 3:4], in_=s_[:, 2:3]), reads=[s_], writes=[s_])
                    fw.op("dve", lambda g: g.scalar_tensor_tensor(out=hv, in0=hv, scalar=s_[:, 3:4], in1=ngB[:, h * 512:(h + 1) * 512], op0=ALU.mult, op1=ALU.mult), reads=[hres_[i], s_, ngB], writes=[hres_[i]])
                    fw.op("pool", lambda g: g.tensor_tensor(out=z_[:], in0=hv, in1=o_[:], op=ALU.mult), reads=[hres_[i], o_], writes=[z_])
                    fw.dma("sp", (ZS[i * 128:(i + 1) * 128, h * 512:(h + 1) * 512], z_[:]), reads=[z_], key=z_.ds)
                hp.close()
            ph.close()
            P2.close()

            ph = Phase(fw)
            gB = [ph.sb("gB%d" % i, [128, D], F32) for i in range(2)]
            build_gb(gB, 32)
            wo = ph.sb("wo", [128, KC, D], BF16)
            wov = ml_w_out[idx].rearrange("(k p) n -> p k n", p=128)
            woh = [Res("wo%d" % q) for q in range(4)]
            for q in range(4):
                fw.dma("pool", (wo[:, :, q * 512:(q + 1) * 512], wov[:, :, q * 512:(q + 1) * 512]), writes=[woh[q]], key=fw.get_dsem())
            l1g = ph.sb("l1g", [128, D], F32)
            l1b = ph.sb("l1b", [128, D], F32)
            fw.dma("sp", (l1g[:], ln1_g[l].partition_broadcast(128)), writes=[l1g], key=l1g.ds)
            fw.dma("sp", (l1b[:], ln1_b[l].partition_broadcast(128)), writes=[l1b], key=l1b.ds)
            zt = [ph.sb("zt%d" % i, [128, D], BF16) for i in range(2)]
            zT = [ph.sb("zT%d" % i, [128, KC, 128], BF16) for i in range(2)]
            xt = [ph.sb("xt%d" % i, [128, D], F32) for i in range(2)]
            yt = [ph.sb("yt%d" % i, [128, D], F32) for i in range(2)]
            jk = ph.sb("jk", [128, D], BF16)
            st2 = [ph.sb("stb%d" % i, [128, 8], F32) for i in range(2)]

            def loadsZ(i):
                fw.dma("sp", (zt[i % 2][:], ZS[i * 128:(i + 1) * 128, :]), writes=[zt[i % 2]], key=zt[i % 2].ds)

            def loadsX5(i):
                load_rows("sp", xt[i % 2], X, l, i)

            def stageA5(i):
                z_, zT_ = zt[i % 2], zT[i % 2]
                for q in range(2):
                    for cc in range(8):
                        c = q * 8 + cc
                        fw.op("pe", lambda g: g.transpose(pbh[:, cc * 128:(cc + 1) * 128], z_[:, c * 128:(c + 1) * 128], identb[:]), reads=[z_, identb], writes=[pbh])
                    fw.op("act", lambda g: g.copy(out=zT_[:, q * 8:(q + 1) * 8, :].rearrange("p c n -> p (c n)"), in_=pbh[:]), reads=[pbh], writes=[zT_])

            nb5 = [0]

            def stageB5(i):
                zT_, x_, y_ = zT[i % 2], xt[i % 2], yt[i % 2]
                yb = []
                for q in range(4):
                    bank = pb[nb5[0] % 7]
                    nb5[0] += 1
                    for k in range(KC):
                        fw.op("pe", lambda g: g.matmul(bank[:], lhsT=zT_[:, k, :], rhs=wo[:, k, q * 512:(q + 1) * 512], start=(k == 0), stop=(k == KC - 1)), reads=[zT_, woh[q]], writes=[bank])
                    yb.append(bank)
                resid_ln(gB, (x_, y_, jk, st2[i % 2]), l, i, yb, l1g, l1b, X)

            for j in range(min(2, ntile_out)):
                loadsZ(j)
                loadsX5(j)
            stageA5(0)
            for i in range(ntile_out):
                if i + 1 < ntile_out:
                    stageA5(i + 1)
                if i + 2 < ntile_out:
                    loadsZ(i + 2)
                stageB5(i)
                if i + 2 < ntile_out:
                    loadsX5(i + 2)
            ph.close()

        phase_ada(0)
        for l in range(nlayers):
            if stop == ("ada", l):
                break
            if l % 2 == 0:
                phase_gmlp(l, l // 2)
            else:
                phase_mlstm(l, l // 2)
            if stop == ("mix", l):
                break
            phase_moe(l)
        fw.barrier()
        G.close()
    return nc


_CACHE = {}


def kernel(**inputs):
    inp = {k: np.ascontiguousarray(np.asarray(v, dtype=np.float32)) for k, v in inputs.items()}
    if "nc" not in _CACHE:
        _CACHE["nc"] = build_program()
    nc = _CACHE["nc"]
    shared = {k: v for k, v in inp.items() if k not in ("x", "c", "ctx", "c_ctx")}
    shared["router_b"] = inp["router_b"].reshape(1, NEXP)
    in_maps = []
    for core in range(8):
        b = core % 4
        m = dict(shared)
        m["x"] = np.ascontiguousarray(inp["x"][b])
        m["c"] = np.ascontiguousarray(inp["c"][b:b + 1])
        m["ctx"] = np.ascontiguousarray(inp["ctx"][b])
        m["c_ctx"] = inp["c_ctx"].reshape(1, D)
        in_maps.append(m)
    res = run_bass_kernel_spmd(nc, in_maps, core_ids=list(range(8)))
    return np.stack([np.asarray(res.results[b]["out"], dtype=np.float32) for b in range(4)], axis=0)
```
